# Optimizing a Trainium2 kernel written in Bass

```python
import math
import jax, jax.numpy as jnp
from jax import lax
import numpy as np

D_MODEL = 1024
BATCH = 2
SEQ = 16384
DEPTH = 4

ML_HEADS = 4
ML_HEAD_DIM = 128
ML_WIDTH = ML_HEADS * ML_HEAD_DIM
ML_CHUNK = 64
CONV_WIDTH = 4
AT_HEADS = 8
AT_HEAD_DIM = 64
AT_WIDTH = AT_HEADS * AT_HEAD_DIM
MIX_WIDTH = ML_WIDTH + AT_WIDTH
MOBA_BLOCK = 256
MOBA_TOPK = 3
MOBA_QCHUNK = 64
EPS = 1e-6
PROJ_SIZES = (2 * ML_WIDTH, ML_WIDTH, ML_WIDTH, ML_WIDTH, ML_HEADS, ML_HEADS, AT_WIDTH, AT_WIDTH, AT_WIDTH, AT_WIDTH)
PROJ_WIDTH = 4 * ML_WIDTH + ML_WIDTH + 2 * ML_HEADS + 4 * AT_WIDTH

kernel_name = 'hybrid_mlstm_moba_alibi'


def _split_points():
    pts, acc = [], 0
    for s in PROJ_SIZES[:-1]:
        acc += s
        pts.append(acc)
    return pts


def _rmsnorm(x, w):
    xf = x.astype(jnp.float32)
    return xf * lax.rsqrt(jnp.mean(xf * xf, axis=-1, keepdims=True) + EPS) * w.astype(jnp.float32)


def _causal_conv(x, w, b):
    K, S = w.shape[0], x.shape[1]
    xp = jnp.pad(x, ((0, 0), (K - 1, 0), (0, 0)))
    return b + sum(xp[:, k:k + S] * w[k] for k in range(K))


def _mlstm_chunkwise(q, k, v, i_pre, f_pre):
    B, S, H, Dh = q.shape
    L = ML_CHUNK
    nc = S // L

    def heads_chunks(t):
        t = t.reshape(B, nc, L, H, *t.shape[3:])
        return jnp.moveaxis(t, 3, 1)

    q = heads_chunks(q) * Dh ** -0.5
    k = heads_chunks(k)
    v = heads_chunks(v)
    ig = heads_chunks(i_pre)
    b = jnp.cumsum(jax.nn.log_sigmoid(heads_chunks(f_pre)), axis=-1)
    b_last = b[..., -1]
    a = b_last[..., None] - b + ig
    m_loc = jnp.max(a, axis=-1)
    w_loc = jnp.exp(a - m_loc[..., None])
    c_loc = jnp.einsum('bhcl,bhcld,bhcle->bhcde', w_loc, k, v)
    n_loc = jnp.einsum('bhcl,bhcld->bhcd', w_loc, k)

    def step(carry, xs):
        c, n, m = carry
        c_l, n_l, m_l, bl = xs
        m_new = jnp.maximum(bl + m, m_l)
        s_prev = jnp.exp(bl + m - m_new)
        s_loc = jnp.exp(m_l - m_new)
        c_new = s_prev[..., None, None] * c + s_loc[..., None, None] * c_l
        n_new = s_prev[..., None] * n + s_loc[..., None] * n_l
        return (c_new, n_new, m_new), (c, n, m)

    init = (jnp.zeros((B, H, Dh, Dh), q.dtype), jnp.zeros((B, H, Dh), q.dtype), jnp.zeros((B, H), q.dtype))
    xs = (jnp.moveaxis(c_loc, 2, 0), jnp.moveaxis(n_loc, 2, 0), jnp.moveaxis(m_loc, 2, 0), jnp.moveaxis(b_last, 2, 0))
    _, (c_prev, n_prev, m_prev) = lax.scan(step, init, xs)
    c_prev = jnp.moveaxis(c_prev, 0, 2)
    n_prev = jnp.moveaxis(n_prev, 0, 2)
    m_prev = jnp.moveaxis(m_prev, 0, 2)

    causal = jnp.tril(jnp.ones((L, L), dtype=bool))
    d = jnp.where(causal, b[..., :, None] - b[..., None, :] + ig[..., None, :], -jnp.inf)
    g = b + m_prev[..., None]
    m_t = jnp.maximum(g, jnp.max(d, axis=-1))
    w_intra = jnp.exp(d - m_t[..., None])
    s_inter = jnp.exp(g - m_t)
    qk = jnp.einsum('bhctd,bhcsd->bhcts', q, k) * w_intra
    num = jnp.einsum('bhcts,bhcse->bhcte', qk, v) + s_inter[..., None] * jnp.einsum('bhctd,bhcde->bhcte', q, c_prev)
    den = jnp.sum(qk, axis=-1) + s_inter * jnp.einsum('bhctd,bhcd->bhct', q, n_prev)
    h = num / jnp.maximum(jnp.abs(den), jnp.exp(-m_t))[..., None]
    return jnp.moveaxis(h, 1, 3).reshape(B, S, H, Dh)


def _moba_attention(q, k, v, slopes):
    B, S, H, Dh = q.shape
    nb = -(-S // MOBA_BLOCK)
    s_pad = nb * MOBA_BLOCK
    pad = ((0, 0), (0, s_pad - S), (0, 0), (0, 0))
    q = jnp.pad(q, pad).transpose(0, 2, 1, 3)
    k = jnp.pad(k, pad).transpose(0, 2, 1, 3)
    v = jnp.pad(v, pad).transpose(0, 2, 1, 3)
    scale = Dh ** -0.5
    kb = k.reshape(B, H, nb, MOBA_BLOCK, Dh)
    vb = v.reshape(B, H, nb, MOBA_BLOCK, Dh)
    k_mean = jnp.mean(kb, axis=3)
    pos = jnp.arange(s_pad)
    gate = jnp.einsum('bhtd,bhnd->bhtn', q, k_mean)
    past = jnp.arange(nb)[None, :] < (pos // MOBA_BLOCK)[:, None]
    gate = jnp.where(past, gate, -jnp.inf)
    topk = min(MOBA_TOPK, nb)
    g_val, sel = lax.top_k(gate, topk)
    valid = jnp.isfinite(g_val)
    nq = s_pad // MOBA_QCHUNK

    def to_chunks(t):
        return jnp.moveaxis(t.reshape(B, H, nq, MOBA_QCHUNK, *t.shape[3:]), 2, 0)

    bi = jnp.arange(B)[:, None, None, None]
    hi = jnp.arange(H)[None, :, None, None]
    offs = jnp.arange(MOBA_BLOCK)

    def one_chunk(args):
        q_c, sel_c, valid_c, ci = args
        q_pos = ci * MOBA_QCHUNK + jnp.arange(MOBA_QCHUNK)
        n_own = (ci * MOBA_QCHUNK) // MOBA_BLOCK
        k_own = lax.dynamic_index_in_dim(kb, n_own, axis=2, keepdims=False)
        v_own = lax.dynamic_index_in_dim(vb, n_own, axis=2, keepdims=False)
        dist_own = q_pos[:, None] - (n_own * MOBA_BLOCK + offs)[None, :]
        s_own = jnp.einsum('bhqd,bhsd->bhqs', q_c, k_own) * scale - slopes[:, None, None] * dist_own
        s_own = jnp.where(dist_own >= 0, s_own, -jnp.inf)
        k_sel = kb[bi, hi, sel_c]
        v_sel = vb[bi, hi, sel_c]
        dist_sel = q_pos[:, None, None] - (sel_c[..., None] * MOBA_BLOCK + offs)
        s_sel = jnp.einsum('bhqd,bhqksd->bhqks', q_c, k_sel) * scale - slopes[:, None, None, None] * dist_sel
        s_sel = jnp.where(valid_c[..., None], s_sel, -jnp.inf)
        s_all = jnp.concatenate([s_sel.reshape(B, H, MOBA_QCHUNK, topk * MOBA_BLOCK), s_own], axis=-1)
        p = jax.nn.softmax(s_all, axis=-1)
        p_sel = p[..., :topk * MOBA_BLOCK].reshape(B, H, MOBA_QCHUNK, topk, MOBA_BLOCK)
        p_own = p[..., topk * MOBA_BLOCK:]
        return jnp.einsum('bhqks,bhqksd->bhqd', p_sel, v_sel) + jnp.einsum('bhqs,bhsd->bhqd', p_own, v_own)

    out = lax.map(one_chunk, (to_chunks(q), to_chunks(sel), to_chunks(valid), jnp.arange(nq)))
    out = jnp.moveaxis(out, 0, 2).reshape(B, H, s_pad, Dh)[:, :, :S]
    return out.transpose(0, 2, 1, 3)


def _layer(x, norm_w, w_in, b_igate, b_fgate, conv_w, conv_b, mlstm_norm_w, q_norm_w, k_norm_w, w_out, slopes):
    B, S, _ = x.shape
    h = _rmsnorm(x, norm_w).astype(x.dtype)
    proj = (h @ w_in).astype(jnp.float32)
    ml_qk, ml_v, ml_o, ml_z, ml_i, ml_f, at_q, at_k, at_v, at_z = jnp.split(proj, _split_points(), axis=-1)

    ml_qk = jax.nn.silu(_causal_conv(ml_qk, conv_w.astype(jnp.float32), conv_b.astype(jnp.float32)))
    ml_q, ml_k = jnp.split(ml_qk, 2, axis=-1)
    hs = (B, S, ML_HEADS, ML_HEAD_DIM)
    h_ml = _mlstm_chunkwise(ml_q.reshape(hs), ml_k.reshape(hs), ml_v.reshape(hs),
                            ml_i + b_igate.astype(jnp.float32), ml_f + b_fgate.astype(jnp.float32))
    h_ml = jax.nn.sigmoid(ml_o).reshape(hs) * h_ml
    h_ml = _rmsnorm(h_ml, mlstm_norm_w.reshape(ML_HEADS, ML_HEAD_DIM))
    y_ml = h_ml.reshape(B, S, ML_WIDTH) * jax.nn.silu(ml_z)

    ha = (B, S, AT_HEADS, AT_HEAD_DIM)
    q = _rmsnorm(at_q.reshape(ha), q_norm_w)
    k = _rmsnorm(at_k.reshape(ha), k_norm_w)
    o_at = _moba_attention(q, k, at_v.reshape(ha), slopes)
    y_at = o_at.reshape(B, S, AT_WIDTH) * jax.nn.silu(at_z)

    y = jnp.concatenate([y_ml, y_at], axis=-1).astype(x.dtype) @ w_out
    return x + y


def setup_inputs(seed: int = 0) -> dict:
    key = jax.random.key(seed)
    ks = jax.random.split(key, 12)
    f32 = jnp.float32
    x = jax.random.normal(ks[0], (BATCH, SEQ, D_MODEL), f32)
    norm_w = 1.0 + 0.05 * jax.random.normal(ks[1], (DEPTH, D_MODEL), f32)
    w_in = jax.random.normal(ks[2], (DEPTH, D_MODEL, PROJ_WIDTH), f32) * D_MODEL ** -0.5
    b_igate = 0.1 * jax.random.normal(ks[3], (DEPTH, ML_HEADS), f32)
    b_fgate = jnp.linspace(3.0, 6.0, ML_HEADS, dtype=f32)[None, :] + 0.1 * jax.random.normal(ks[4], (DEPTH, ML_HEADS), f32)
    conv_w = jax.random.normal(ks[5], (DEPTH, CONV_WIDTH, 2 * ML_WIDTH), f32) * CONV_WIDTH ** -0.5
    conv_b = 0.02 * jax.random.normal(ks[6], (DEPTH, 2 * ML_WIDTH), f32)
    mlstm_norm_w = 1.0 + 0.05 * jax.random.normal(ks[7], (DEPTH, ML_WIDTH), f32)
    q_norm_w = 1.0 + 0.05 * jax.random.normal(ks[8], (DEPTH, AT_HEAD_DIM), f32)
    k_norm_w = 1.0 + 0.05 * jax.random.normal(ks[9], (DEPTH, AT_HEAD_DIM), f32)
    w_out = jax.random.normal(ks[10], (DEPTH, MIX_WIDTH, D_MODEL), f32) * MIX_WIDTH ** -0.5
    return {'x': x, 'norm_w': norm_w, 'w_in': w_in, 'b_igate': b_igate, 'b_fgate': b_fgate,
            'conv_w': conv_w, 'conv_b': conv_b, 'mlstm_norm_w': mlstm_norm_w,
            'q_norm_w': q_norm_w, 'k_norm_w': k_norm_w, 'w_out': w_out}


def reference(x, norm_w, w_in, b_igate, b_fgate, conv_w, conv_b, mlstm_norm_w, q_norm_w, k_norm_w, w_out):
    slopes = 2.0 ** (-8.0 * (jnp.arange(AT_HEADS, dtype=jnp.float32) + 1.0) / AT_HEADS)
    for l in range(DEPTH):
        x = _layer(x, norm_w[l], w_in[l], b_igate[l], b_fgate[l], conv_w[l], conv_b[l],
                   mlstm_norm_w[l], q_norm_w[l], k_norm_w[l], w_out[l], slopes)
    return x
```

```python
import numpy as np
import ml_dtypes
from contextlib import ExitStack
import concourse.bass as bass
import concourse.mybir as mybir
from concourse.bass_utils import run_bass_kernel_spmd

F32 = mybir.dt.float32
BF16 = mybir.dt.bfloat16
AF = mybir.ActivationFunctionType
ALU = mybir.AluOpType
AX = mybir.AxisListType

D_MODEL = 1024
DEPTH = 4
AT_HEADS = 8
EPS = 1e-6
PROJ_W = 1154
NF, NA, NB_ = 256, 386, 512
CUT = 80.0
NEG = 30000.0
ND = 128
SLOPES = [2.0 ** (-8.0 * (h + 1.0) / AT_HEADS) for h in range(AT_HEADS)]


def _layout(items):
    off, o = {}, 0
    for n, w in items:
        off[n] = (o, o + w)
        o += w
    return off, o


CST_OFF, CST_N = _layout([('normw', 8), ('convw', 8), ('convb', 2), ('bf', 1), ('bi', 1), ('eps', 1), ('one', 1),
                          ('mlw', 128), ('wq', 64), ('wk', 64), ('identf', 128), ('tri', 128), ('blk', 128),
                          ('sel0', 128), ('sel1', 128), ('alibi', 2 * ND)])
CSTB_OFF, CSTB_N = _layout([('identb', 128), ('onesb', 128), ('cmml', 128), ('cm0', 256), ('cm1', 256)])


class Buf:
    __slots__ = ("name", "w", "r", "wsem", "wcnt")

    def __init__(self, name):
        self.name = name
        self.w = None
        self.r = []
        self.wsem = None
        self.wcnt = 0


class Sync:
    ROT = 30000

    def __init__(self, nc, es):
        self.nc = nc
        self.es = es
        self.engs = {'pe': nc.tensor, 'act': nc.scalar, 'dve': nc.vector, 'pool': nc.gpsimd, 'sp': nc.sync}
        self.sem = {}
        self.cnt = {}
        self.waited = {e: {} for e in self.engs}
        self.nsem = 0
        for e in self.engs:
            self.sem[e] = self.newsem('prog_' + e)
            self.cnt[e] = 0
        self.ninst = 0
        self.bufs = {}
        self.alias = {}
        self.excl = set()

    def b(self, n):
        n = self.alias.get(n, n)
        if n not in self.bufs:
            self.bufs[n] = Buf(n)
        return self.bufs[n]

    def newsem(self, name):
        self.nsem += 1
        return self.es.enter_context(self.nc.semaphore(name + '_%d' % self.nsem))

    def _emit_waits(self, e, toks):
        need = {}
        for tok in toks:
            if tok is None:
                continue
            sem, val = tok
            k = id(sem)
            if self.waited[e].get(k, 0) >= val:
                continue
            if k not in need or need[k][1] < val:
                need[k] = (sem, val)
        for k, (sem, val) in need.items():
            self.waited[e][k] = val
            self.engs[e].wait_ge(sem, val)

    def _deps(self, e, reads, writes):
        toks = []
        for b in reads:
            toks.append(b.w)
        for b in writes:
            toks.append(b.w)
            toks.extend(b.r)
        self._emit_waits(e, toks)

    def _bl(self, names):
        return [self.b(n) if isinstance(n, str) else n for n in names]

    def op(self, e, fn, reads=(), writes=()):
        reads = self._bl(reads)
        writes = self._bl(writes)
        xr = [b for b in reads if b.name in self.excl and b not in writes]
        if xr:
            writes = writes + xr
        self._deps(e, reads, writes)
        ins = fn(self.engs[e])
        if self.cnt[e] >= self.ROT:
            self.sem[e] = self.newsem('prog_' + e)
            self.cnt[e] = 0
        self.cnt[e] += 1
        ins.then_inc(self.sem[e], 1)
        tok = (self.sem[e], self.cnt[e])
        for b in reads:
            b.r.append(tok)
        for b in writes:
            b.w = tok
            b.r = []
        self.ninst += 1
        return ins

    def dma(self, q, out_ap, in_ap, src, dst, **kw):
        src, dst = self._bl([src, dst])
        self._deps(q, [src], [dst])
        if dst.wsem is None or dst.wcnt >= self.ROT:
            dst.wsem = self.newsem('w_' + dst.name)
            dst.wcnt = 0
        ins = self.engs[q].dma_start(out=out_ap, in_=in_ap, **kw)
        dst.wcnt += 16
        ins.then_inc(dst.wsem, 16)
        tok = (dst.wsem, dst.wcnt)
        src.r.append(tok)
        dst.w = tok
        dst.r = []
        self.ninst += 1
        return ins

    def finish(self, names):
        toks = []
        for b in self._bl(names):
            toks.append(b.w)
            toks.extend(b.r)
        for b in self.bufs.values():
            if b.wsem is not None:
                toks.append((b.wsem, b.wcnt))
        self._emit_waits('sp', toks)


STOP = None


class EarlyStop(Exception):
    pass


def ck(name):
    if STOP == name:
        raise EarlyStop()


CUT_SLOPE = (SLOPES[3], SLOPES[7])


def _visit_tiles(hh, qb):
    slope = CUT_SLOPE[hh]
    out = []
    for kt in range(0, 2 * qb):
        delta = 2 * qb - kt
        if slope * (delta * 128 - 127) > CUT:
            continue
        out.append(kt)
    return out


def build_fused(S, depth):
    NT = S // 128
    NST = S // 512
    YW = min(4096, S)
    XW = min(2048, S)
    NCY = S // YW
    NCX = S // XW
    groups = [[0, 1, 2, 3], [4, 5, 6, 7]]
    nc = bass.Bass("TRN2", target_bir_lowering=False)
    xT_d = nc.dram_tensor("xT", [8, 128, S], F32, kind="ExternalInput").ap()
    xo_d = nc.dram_tensor("xo", [2, 128, S], F32, kind="ExternalInput").ap()
    w_d = nc.dram_tensor("w", [depth, 8, 128, PROJ_W], F32, kind="ExternalInput").ap()
    cst_d = nc.dram_tensor("cst", [depth, 128, CST_N], F32, kind="ExternalInput").ap()
    wo_d = nc.dram_tensor("wo", [depth, 8, 128, 256], F32, kind="ExternalInput").ap()
    cstb_d = nc.dram_tensor("cstb", [128, CSTB_N], BF16, kind="ExternalInput").ap()
    oh_d = nc.dram_tensor("oh", [33, S], BF16, kind="ExternalInput").ap()
    alr_d = nc.dram_tensor("alr", [2, 256], BF16, kind="ExternalInput").ap()
    xn_d = nc.dram_tensor("xn", [2, 128, S], F32, kind="ExternalOutput").ap()
    yp1 = [[nc.dram_tensor("yp_%d_%d" % (hf, cc), [128, YW], BF16) for cc in range(NCY)] for hf in range(2)]
    yg1 = [[nc.dram_tensor("yg_%d_%d" % (hf, cc), [512, YW], BF16) for cc in range(NCY)] for hf in range(2)]
    xp2 = [[[nc.dram_tensor("xp%d_%d_%d" % (q, m, cc), [128, XW], F32) for cc in range(NCX)] for m in range(2)] for q in range(2)]
    xg1 = [[nc.dram_tensor("xg_%d_%d" % (m, cc), [512, XW], F32) for cc in range(NCX)] for m in range(2)]
    yp = [yp1] * depth
    yg = [yg1] * depth
    xp = [xp2[l % 2] for l in range(depth)]
    xg = [xg1] * depth

    with ExitStack() as es:
        def sb(name, shape, dt):
            return es.enter_context(nc.sbuf_tensor("s_" + name, shape, dt))

        def ps(name, shape, dt):
            return es.enter_context(nc.psum_tensor("p_" + name, shape, dt))

        K = Sync(nc, es)
        K.alias = {'TB0': 'TB', 'TB1': 'TB', 'TB2': 'TB', 'TB30': 'TB', 'TB31': 'TB', 'TBm0': 'TB', 'TBm1': 'TB',
                   'MSg': 'MS', 'MSs': 'MS', 'MSd0': 'MS', 'MSd1': 'MS', 'MSgate': 'MS',
                   'AS0': 'AS', 'AS1': 'AS', 'AOt0': 'AO', 'AOt1': 'AO'}
        K.excl = {'PF', 'PA', 'PB', 'TB', 'MS', 'MA', 'AS', 'AO'}
        cst = sb("cst", [128, CST_N], F32)
        cstb = sb("cstb", [128, CSTB_N], BF16)
        Wb = sb("Wb", [128, 8, PROJ_W], BF16)
        XT = sb("XT", [128, 8, 512], F32)
        SQ = sb("SQ", [128, 2, 512], BF16)
        HT = sb("HT", [128, 8, 512], BF16)
        RS = sb("RS", [128, 512], F32)
        XC = [sb("XC%d" % i, [128, 515], F32) for i in range(2)]
        CA = [sb("CA%d" % i, [128, 512], F32) for i in range(2)]
        SG = sb("SG", [128, 512], F32)
        QT = sb("QT", [128, 512], BF16)
        KT = sb("KT", [128, 512], BF16)
        VML = sb("VML", [128, 4, 129], BF16)
        SO = sb("SO", [128, 4, 128], F32)
        SZ = sb("SZ", [128, 4, 128], F32)
        ZW = sb("ZW", [128, 4, 128], F32)
        IFt = sb("IFt", [128, 2, 4], F32)
        GT = sb("GT", [128, 8, 4], F32)
        WK = sb("WK", [128, 4], F32)
        WQ = sb("WQ", [128, 4], F32)
        SC = sb("SC", [128, 8], F32)
        KK = sb("KK", [128, 128], BF16)
        PT = sb("PT", [128, 128], BF16)
        Cst = sb("Cst", [128, 129], F32)
        CP = sb("CP", [128, 129], F32)
        CB = [sb("CB%d" % i, [128, 129], BF16) for i in range(2)]
        SM = sb("SM", [128, 8], F32)
        HG = sb("HG", [128, 128], F32)
        JK = sb("JK", [128, 128], F32)
        YT = sb("YT", [128, 4, 256], BF16)
        YTT = sb("YTT", [128, 2, 512], BF16)
        SS = sb("SS", [128, 4], F32)
        QN = sb("QN", [128, 2, 2, 2, 96], BF16)
        KN = sb("KN", [128, 64], BF16)
        SZA = sb("SZA", [128, 4, 128], F32)
        KTA = [sb("KTA%d" % i, [97, S], BF16) for i in range(2)]
        VAT = [sb("VAT%d" % i, [128, NT, 65], BF16) for i in range(2)]
        KM = [sb("KM%d" % i, [64, 64], BF16) for i in range(2)]
        KMS = sb("KMS", [64, 2], F32)
        QTA = [[sb("QTA%d%d" % (i, v), [97, 256], BF16) for v in range(2)] for i in range(2)]
        G = sb("G", [128, 64], F32)
        T8 = sb("T8", [128, 8], F32)
        PTA = [sb("PTA%d" % i, [128, 256], BF16) for i in range(3)]
        OT = sb("OT", [65, 256], F32)
        AM = sb("AM", [128, 4], F32)
        PF = ps("PF", [128, 512], F32)
        PA = ps("PA", [128, 512], F32)
        PB = ps("PB", [128, 512], F32)
        TB = ps("TB", [128, 1024], BF16)
        MS = ps("MS", [128, 512], F32)
        MA = ps("MA", [128, 512], F32)
        AS = ps("AS", [128, 512], F32)
        AO = ps("AO", [128, 512], F32)

        def C(name):
            a, b_ = CST_OFF[name]
            return cst[:, a:b_]

        def CBV(name):
            a, b_ = CSTB_OFF[name]
            return cstb[:, a:b_]

        identf = C('identf')
        identb = CBV('identb')
        onesb = CBV('onesb')
        eps_ap = C('eps')[:, 0:1]
        Wst = XT[:].rearrange("p c t -> p (c t)")[:, 0:PROJ_W]

        K.dma('sp', cstb[:], cstb_d[:, :], 'd_cstb', 'cstb')
        for i in range(2):
            K.dma('sp', KTA[i][64:97, :], oh_d[:, :], 'd_oh', 'KTAc%d' % i)
            for v in range(2):
                K.dma('sp', QTA[i][v][96:97, :], alr_d[i:i + 1, :], 'd_alr', 'QTAc%d%d' % (i, v))
            K.op('pool', lambda e, i=i: e.memset(VAT[i][:], 1.0), writes=['VATones%d' % i])
        K.op('pool', lambda e: e.memset(VML[:], 1.0), writes=['VMLones'])
        for i in range(2):
            for kt in range(NT):
                K.b('VAT%d_%d' % (i, kt)).w = K.b('VATones%d' % i).w
        for j in range(4):
            K.b('VML%d' % j).w = K.b('VMLones').w

        def collective(in_t, out_t, src, dst):
            src, dst = K._bl([src, dst])
            K._deps('pool', [src], [dst])
            if dst.wsem is None:
                dst.wsem = K.newsem('cc_' + dst.name)
                dst.wcnt = 0
            ins = nc.gpsimd.collective_compute("AllGather", ALU.bypass, replica_groups=groups,
                                               ins=[in_t.ap().opt()], outs=[out_t.ap().opt()])
            dst.wcnt += 1
            ins.then_inc(dst.wsem)
            tok = (dst.wsem, dst.wcnt)
            src.r.append(tok)
            dst.w = tok
            dst.r = []

        QSCALE = 128.0 ** -0.5

        def attention(st, qbl):
            qb = st * 2 + qbl
            for hh in range(2):
                visit = _visit_tiles(hh, qb)
                tiles = visit + [2 * qb, 2 * qb + 1]
                variants = sorted(set((kt // 2) // 32 for kt in tiles))
                if qb >= 4:
                    ncol = max(8, qb)
                    for sub in range(2):
                        qnb = 'QN%d%d' % (hh, sub)
                        K.op('pe', lambda e, sub=sub: e.matmul(MS[:, 416:416 + qb], QTA[hh][0][0:64, sub * 128:(sub + 1) * 128],
                                                               KM[hh][:, 0:qb], start=True, stop=True),
                             reads=['QTAq%d0' % hh] + ['KM%d_%d' % (hh, bl) for bl in range(qb)], writes=['MSgate'])
                        K.op('dve', lambda e: e.tensor_copy(G[:, 0:qb], MS[:, 416:416 + qb]), reads=['MSgate'], writes=['G'])
                        K.op('dve', lambda e: e.max(T8[:], G[:, 0:ncol]), reads=['G'], writes=['T8'])
                        for v in variants:
                            K.op('dve', lambda e, sub=sub, v=v: e.tensor_scalar(QN[:, hh, sub, v, 64:96], G[:, 32 * v:32 * v + 32],
                                                                               T8[:, 2:3], None, ALU.is_lt),
                                 reads=['G', 'T8'], writes=[qnb])
                        vo, co = qb // 32, qb % 32
                        K.op('dve', lambda e, sub=sub: e.memset(QN[:, hh, sub, vo, 64 + co:65 + co], 0.0), writes=[qnb])
                        for v in variants:
                            K.op('pe', lambda e, sub=sub, v=v: e.transpose(TB[0:96, 512 + v * 128:640 + v * 128], QN[:, hh, sub, v, :], identb),
                                 reads=[qnb, 'cstb'], writes=['TBm%d' % v])
                            K.op('dve', lambda e, sub=sub, v=v: e.tensor_copy(QTA[hh][v][64:96, sub * 128:(sub + 1) * 128],
                                                                             TB[64:96, 512 + v * 128:640 + v * 128]),
                                 reads=['TBm%d' % v], writes=['QTAm%d%d' % (hh, v)])
                nt_ = len(tiles)
                for idx, kt in enumerate(tiles):
                    v = (kt // 2) // 32
                    slot = idx % 2
                    own = kt >= 2 * qb
                    asb = 'AS%d' % slot
                    K.op('pe', lambda e, kt=kt, v=v, slot=slot, own=own: e.matmul(
                        AS[:, slot * 256:(slot + 1) * 256], KTA[hh][0:97, kt * 128:(kt + 1) * 128], QTA[hh][v][0:97, 0:256],
                        start=True, stop=not own),
                         reads=['KTA%d_%d' % (hh, kt), 'KTAc%d' % hh, 'QTAq%d%d' % (hh, v), 'QTAm%d%d' % (hh, v), 'QTAc%d%d' % (hh, v)],
                         writes=[asb])
                    if own:
                        cmn = 'cm0' if kt == 2 * qb else 'cm1'
                        K.op('pe', lambda e, slot=slot, cmn=cmn: e.matmul(AS[:, slot * 256:(slot + 1) * 256], identb, CBV(cmn),
                                                                         start=False, stop=True),
                             reads=['cstb'], writes=[asb])
                    delta = 2 * qb - kt
                    col = CST_OFF['alibi'][0] + hh * ND + delta + 1
                    pi = idx % 3
                    K.op('act', lambda e, slot=slot, col=col, pi=pi: e.activation(PTA[pi][:], AS[:, slot * 256:(slot + 1) * 256], AF.Exp,
                                                                                  bias=cst[:, col:col + 1]),
                         reads=[asb, 'cst'], writes=['PTA%d' % pi])
                    K.op('pe', lambda e, kt=kt, pi=pi, idx=idx: e.matmul(AO[0:65, 0:256], VAT[hh][:, kt, :], PTA[pi][:],
                                                                        start=(idx == 0), stop=(idx == nt_ - 1)),
                         reads=['VAT%d_%d' % (hh, kt), 'PTA%d' % pi], writes=['AO'])
                K.op('act', lambda e: e.copy(OT[:], AO[0:65, 0:256]), reads=['AO'], writes=['OT'])
                for sub in range(2):
                    j = 2 * qbl + sub
                    c0 = 256 + sub * 128
                    K.op('pe', lambda e, sub=sub, c0=c0: e.transpose(AO[:, c0:c0 + 65], OT[:, sub * 128:(sub + 1) * 128], identf[0:65, 0:65]),
                         reads=['OT', 'cst'], writes=['AOt%d' % sub])
                    K.op('dve', lambda e, sub=sub, c0=c0: e.reciprocal(AM[:, sub:sub + 1], AO[:, c0 + 64:c0 + 65]),
                         reads=['AOt%d' % sub], writes=['AM%d' % sub])
                    K.op('dve', lambda e, sub=sub, c0=c0, j=j: e.scalar_tensor_tensor(
                        YT[:, j, 128 + hh * 64:128 + (hh + 1) * 64], AO[:, c0:c0 + 64], AM[:, sub:sub + 1],
                        SZA[:, j, hh * 64:(hh + 1) * 64], ALU.mult, ALU.mult),
                         reads=['AOt%d' % sub, 'AM%d' % sub, 'SZA%d' % j], writes=['YTa%d' % j])

        for l in range(depth):
            K.dma('sp', cst[:], cst_d[l, :, :], 'd_cst', 'cst')
            for c in range(8):
                K.dma('sp', Wst, w_d[l, c, :, :], 'd_w', 'XT')
                K.op('dve', lambda e, c=c: e.tensor_scalar(Wb[:, c, :], Wst, C('normw')[:, c:c + 1], None, ALU.mult),
                     reads=['XT', 'cst'], writes=['Wb'])
            for i in range(2):
                for v in range(2):
                    K.op('pool', lambda e, i=i, v=v: e.memset(QTA[i][v][64:96, :], 0.0), writes=['QTAm%d%d' % (i, v)])
                K.op('pool', lambda e, i=i: e.memset(XC[i][:, 0:3], 0.0), writes=['XCh%d' % i])
            K.op('pool', lambda e: e.memset(Cst[:], 0.0), writes=['Cst'])
            K.op('pool', lambda e: e.memset(G[:], -1e30), writes=['G'])
            K.op('pool', lambda e: e.memset(QN[:], 0.0), writes=['QN00', 'QN01', 'QN10', 'QN11'])

            try:
                ck('setup')
                for st in range(NST):
                    t0 = st * 512
                    if l == 0:
                        K.dma('sp', XT[:], xT_d.rearrange("c p t -> p c t")[:, :, t0:t0 + 512], 'd_x', 'XT')
                    else:
                        ccx, offx = t0 // XW, t0 % XW
                        for m in range(2):
                            K.dma('sp', XT[:].rearrange("p (r h) t -> p h r t", h=2)[:, m, :, :],
                                  xg[l - 1][m][ccx].ap().rearrange("(r p) t -> p r t", p=128)[:, :, offx:offx + 512], 'xg', 'XT')
                    for c in range(8):
                        sl = c % 2
                        K.op('act', lambda e, c=c, sl=sl: e.activation(SQ[:, sl, :], XT[:, c, :], AF.Square), reads=['XT'], writes=['SQ%d' % sl])
                        K.op('pe', lambda e, c=c, sl=sl: e.matmul(PF[:], onesb, SQ[:, sl, :], start=(c == 0), stop=(c == 7)),
                             reads=['SQ%d' % sl, 'cstb'], writes=['PF'])
                    K.op('act', lambda e: e.activation(RS[:], PF[:], AF.Sqrt, bias=eps_ap, scale=1.0 / D_MODEL),
                         reads=['PF', 'cst'], writes=['RS'])
                    K.op('dve', lambda e: e.reciprocal(RS[:], RS[:]), reads=['RS'], writes=['RS'])
                    for c in range(8):
                        eng = 'dve' if c % 2 == 0 else 'pool'
                        K.op(eng, lambda e, c=c: e.tensor_tensor(HT[:, c, :], XT[:, c, :], RS[:], ALU.mult),
                             reads=['XT', 'RS'], writes=['HT%d' % c])
                    HTb = ['HT%d' % c for c in range(8)]
                    ck('norm')
                    cw = C('convw')
                    cb = C('convb')
                    for qk in range(2):
                        for c in range(8):
                            K.op('pe', lambda e, c=c, qk=qk: e.matmul(PF[:], Wb[:, c, qk * 128:(qk + 1) * 128], HT[:, c, :],
                                                                     start=(c == 0), stop=(c == 7)),
                                 reads=['Wb', HTb[c]], writes=['PF'])
                        K.op('act', lambda e, qk=qk: e.copy(XC[qk][:, 3:515], PF[:]), reads=['PF'], writes=['XCb%d' % qk])
                        K.op('dve', lambda e, qk=qk: e.tensor_scalar(CA[qk][:], XC[qk][:, 3:515], cw[:, qk * 4 + 3:qk * 4 + 4],
                                                                     cb[:, qk:qk + 1], ALU.mult, ALU.add),
                             reads=['XCb%d' % qk, 'cst'], writes=['CA%d' % qk])
                        for k in (2, 1, 0):
                            K.op('dve', lambda e, qk=qk, k=k: e.scalar_tensor_tensor(CA[qk][:], XC[qk][:, k:k + 512],
                                                                                    cw[:, qk * 4 + k:qk * 4 + k + 1], CA[qk][:],
                                                                                    ALU.mult, ALU.add),
                                 reads=['XCb%d' % qk, 'XCh%d' % qk, 'cst', 'CA%d' % qk], writes=['CA%d' % qk])
                        K.op('pool', lambda e, qk=qk: e.tensor_copy(XC[qk][:, 0:3], XC[qk][:, 512:515]),
                             reads=['XCb%d' % qk], writes=['XCh%d' % qk])
                        if qk == 0:
                            K.op('act', lambda e: e.activation(SG[:], CA[0][:], AF.Sigmoid), reads=['CA0'], writes=['SG'])
                            K.op('dve', lambda e: e.scalar_tensor_tensor(QT[:], CA[0][:], QSCALE, SG[:], ALU.mult, ALU.mult),
                                 reads=['CA0', 'SG'], writes=['QT'])
                        else:
                            K.op('act', lambda e: e.activation(KT[:], CA[1][:], AF.Silu), reads=['CA1'], writes=['KT'])
                    ck('conv')
                    for j in range(4):
                        tg = st * 4 + j
                        for c in range(8):
                            K.op('pe', lambda e, c=c, j=j: e.matmul(PA[:, 0:NA], HT[:, c, j * 128:(j + 1) * 128],
                                                                   Wb[:, c, NF:NF + NA], start=(c == 0), stop=(c == 7)),
                                 reads=['Wb', HTb[c]], writes=['PA'])
                        for c in range(8):
                            K.op('pe', lambda e, c=c, j=j: e.matmul(PB[:, 0:NB_], HT[:, c, j * 128:(j + 1) * 128],
                                                                   Wb[:, c, NF + NA:PROJ_W], start=(c == 0), stop=(c == 7)),
                                 reads=['Wb', HTb[c]], writes=['PB'])
                        ck('pA')
                        K.op('act', lambda e, j=j: e.copy(VML[:, j, 0:128], PA[:, 0:128]), reads=['PA'], writes=['VML%d' % j])
                        K.op('act', lambda e, j=j: e.activation(SO[:, j, :], PA[:, 128:256], AF.Sigmoid), reads=['PA'], writes=['SO%d' % j])
                        K.op('act', lambda e, j=j: e.activation(SZ[:, j, :], PA[:, 256:384], AF.Silu), reads=['PA'], writes=['SZ%d' % j])
                        K.op('pool', lambda e, j=j: e.tensor_tensor(ZW[:, j, :], SZ[:, j, :], C('mlw'), ALU.mult),
                             reads=['SZ%d' % j, 'cst'], writes=['ZW%d' % j])
                        K.op('dve', lambda e, j=j: e.tensor_copy(IFt[:, :, j], PA[:, 384:386]), reads=['PA'], writes=['IFt'])
                        ck('evA')
                        sub = j % 2
                        for hh in range(2):
                            cb0 = hh * 256
                            qnb = 'QN%d%d' % (hh, sub)
                            K.op('act', lambda e, cb0=cb0: e.activation(JK[:, 0:64], PB[:, cb0:cb0 + 64], AF.Square, accum_out=SS[:, 0:1]),
                                 reads=['PB'], writes=['JK', 'SS'])
                            K.op('act', lambda e, cb0=cb0: e.activation(JK[:, 64:128], PB[:, cb0 + 64:cb0 + 128], AF.Square, accum_out=SS[:, 1:2]),
                                 reads=['PB'], writes=['JK', 'SS'])
                            K.op('act', lambda e: e.activation(SS[:, 2:4], SS[:, 0:2], AF.Sqrt, bias=eps_ap, scale=1.0 / 64),
                                 reads=['SS', 'cst'], writes=['SS'])
                            K.op('dve', lambda e: e.reciprocal(SS[:, 2:4], SS[:, 2:4]), reads=['SS'], writes=['SS'])
                            K.op('dve', lambda e: e.tensor_scalar(SS[:, 2:3], SS[:, 2:3], 0.125, None, ALU.mult), reads=['SS'], writes=['SS'])
                            ck('evB1')
                            K.op('dve', lambda e, cb0=cb0, hh=hh, sub=sub: e.scalar_tensor_tensor(
                                QN[:, hh, sub, 0, 0:64], PB[:, cb0:cb0 + 64], SS[:, 2:3], C('wq'), ALU.mult, ALU.mult),
                                 reads=['PB', 'SS', 'cst'], writes=[qnb])
                            K.op('dve', lambda e, cb0=cb0: e.scalar_tensor_tensor(
                                KN[:], PB[:, cb0 + 64:cb0 + 128], SS[:, 3:4], C('wk'), ALU.mult, ALU.mult),
                                 reads=['PB', 'SS', 'cst'], writes=['KN'])
                            K.op('act', lambda e, cb0=cb0, hh=hh, tg=tg: e.copy(VAT[hh][:, tg, 0:64], PB[:, cb0 + 128:cb0 + 192]),
                                 reads=['PB'], writes=['VAT%d_%d' % (hh, tg)])
                            K.op('act', lambda e, cb0=cb0, hh=hh, j=j: e.activation(SZA[:, j, hh * 64:(hh + 1) * 64],
                                                                                    PB[:, cb0 + 192:cb0 + 256], AF.Silu),
                                 reads=['PB'], writes=['SZA%d' % j])
                            ck('evB2')
                            K.op('pe', lambda e, hh=hh, sub=sub: e.transpose(TB[0:64, 0:128], QN[:, hh, sub, 0, 0:64], identb),
                                 reads=[qnb, 'cstb'], writes=['TB0'])
                            ck('t1')
                            K.op('act', lambda e, hh=hh, sub=sub: e.copy(QTA[hh][0][0:64, sub * 128:(sub + 1) * 128], TB[0:64, 0:128]),
                                 reads=['TB0'], writes=['QTAq%d0' % hh])
                            ck('t2')
                            K.op('pool', lambda e, hh=hh, sub=sub: e.tensor_copy(QTA[hh][1][0:64, sub * 128:(sub + 1) * 128],
                                                                                  QTA[hh][0][0:64, sub * 128:(sub + 1) * 128]),
                                 reads=['QTAq%d0' % hh], writes=['QTAq%d1' % hh])
                            ck('t3')
                            K.op('pe', lambda e: e.transpose(TB[0:64, 128:256], KN[:], identb), reads=['KN', 'cstb'], writes=['TB1'])
                            ck('t4')
                            K.op('dve', lambda e, hh=hh, tg=tg: e.tensor_copy(KTA[hh][0:64, tg * 128:(tg + 1) * 128], TB[0:64, 128:256]),
                                 reads=['TB1'], writes=['KTA%d_%d' % (hh, tg)])
                            ck('evB3')
                            if sub == 1:
                                blk = tg // 2
                                K.op('dve', lambda e, hh=hh, blk=blk: e.reduce_sum(KMS[:, hh:hh + 1], KTA[hh][0:64, blk * 256:(blk + 1) * 256], AX.X),
                                     reads=['KTA%d_%d' % (hh, tg - 1), 'KTA%d_%d' % (hh, tg)], writes=['KMS%d' % hh])
                                K.op('act', lambda e, hh=hh, blk=blk: e.mul(KM[hh][:, blk:blk + 1], KMS[:, hh:hh + 1], 1.0 / 256),
                                     reads=['KMS%d' % hh], writes=['KM%d_%d' % (hh, blk)])
                        if sub == 1:
                            ck('proj')
                            attention(st, j // 2)
                            ck('attn')

                    K.op('dve', lambda e: e.tensor_scalar(GT[:, 0, :], IFt[:, 1, :], C('bf')[:, 0:1], None, ALU.add),
                         reads=['IFt', 'cst'], writes=['GT'])
                    K.op('act', lambda e: e.activation(GT[:, 1, :], GT[:, 0, :], AF.Exp, scale=-1.0), reads=['GT'], writes=['GT'])
                    K.op('act', lambda e: e.activation(GT[:, 2, :], GT[:, 1, :], AF.Ln, bias=C('one')[:, 0:1]), reads=['GT', 'cst'],
                         writes=['GT'])
                    for i, nm in enumerate(('tri', 'blk', 'sel0', 'sel1')):
                        K.op('pe', lambda e, i=i, nm=nm: e.matmul(MS[:, 392 + 4 * i:396 + 4 * i], C(nm), GT[:, 2, :], start=True, stop=True),
                             reads=['GT', 'cst'], writes=['MSg'])
                    K.op('dve', lambda e: e.tensor_tensor(GT[:, 3, :], IFt[:, 0, :], MS[:, 392:396], ALU.add),
                         reads=['IFt', 'MSg'], writes=['GT'])
                    K.op('dve', lambda e: e.tensor_tensor(GT[:, 3, :], GT[:, 3, :], MS[:, 396:400], ALU.subtract),
                         reads=['MSg', 'GT'], writes=['GT'])
                    K.op('act', lambda e: e.activation(WK[:], GT[:, 3, :], AF.Exp, bias=C('bi')[:, 0:1]), reads=['GT', 'cst'], writes=['WK'])
                    K.op('dve', lambda e: e.tensor_copy(GT[:, 4, :], MS[:, 396:400]), reads=['MSg'], writes=['GT'])
                    K.op('dve', lambda e: e.tensor_tensor(GT[:, 4, :], GT[:, 4, :], MS[:, 392:396], ALU.subtract),
                         reads=['MSg', 'GT'], writes=['GT'])
                    K.op('act', lambda e: e.activation(WQ[:], GT[:, 4, :], AF.Exp), reads=['GT'], writes=['WQ'])
                    K.op('act', lambda e: e.activation(SC[:].rearrange("p (j h) -> p h j", h=2),
                                                       MS[:, 400:408].rearrange("p (h j) -> p h j", h=2), AF.Exp, scale=-1.0),
                         reads=['MSg'], writes=['SC'])

                    ck('gates')
                    for j in range(4):
                        K.op('pe', lambda e, j=j: e.transpose(TB[:, 256:384], KT[:, j * 128:(j + 1) * 128], identb),
                             reads=['KT', 'cstb'], writes=['TB2'])
                        K.op('dve', lambda e, j=j: e.tensor_scalar(KK[:], TB[:, 256:384], WK[:, j:j + 1], None, ALU.mult),
                             reads=['TB2', 'WK'], writes=['KK'])
                        K.op('pe', lambda e, j=j: e.matmul(MS[:, 0:128], KT[:, j * 128:(j + 1) * 128], QT[:, j * 128:(j + 1) * 128],
                                                           start=True, stop=True),
                             reads=['KT', 'QT'], writes=['MSs'])
                        K.op('dve', lambda e, j=j: e.scalar_tensor_tensor(PT[:], MS[:, 0:128], WK[:, j:j + 1], CBV('cmml'), ALU.mult, ALU.mult),
                             reads=['MSs', 'WK', 'cstb'], writes=['PT'])
                        K.op('pe', lambda e, j=j: e.matmul(MA[:, 0:129], PT[:], VML[:, j, :], start=True, stop=False),
                             reads=['PT', 'VML%d' % j], writes=['MA'])
                        for h in range(2):
                            ci = 2 * j + h
                            lo, hi = h * 64, (h + 1) * 64
                            d0 = 128 + 130 * h
                            K.op('dve', lambda e, ci=ci: e.tensor_scalar(CP[:], Cst[:], SC[:, ci:ci + 1], None, ALU.mult),
                                 reads=['Cst', 'SC'], writes=['CP'])
                            K.op('act', lambda e, h=h: e.copy(CB[h][:], CP[:]), reads=['CP'], writes=['CB%d' % h])
                            K.op('pe', lambda e, j=j, h=h, lo=lo, hi=hi: e.matmul(MA[lo:hi, 0:129], QT[:, j * 128 + lo:j * 128 + hi], CB[h][:],
                                                                               start=False, stop=True),
                                 reads=['QT', 'CB%d' % h], writes=['MA'])
                            K.op('pe', lambda e, j=j, lo=lo, hi=hi, d0=d0: e.matmul(MS[:, d0:d0 + 129], KK[lo:hi, :], VML[lo:hi, j, :],
                                                                                 start=True, stop=True),
                                 reads=['KK', 'VML%d' % j], writes=['MSd%d' % h])
                            K.op('dve', lambda e, d0=d0: e.tensor_tensor(Cst[:], CP[:], MS[:, d0:d0 + 129], ALU.add),
                                 reads=['CP', 'MSd%d' % h], writes=['Cst'])
                        K.op('dve', lambda e, j=j: e.tensor_scalar(SM[:, 6:7], MA[:, 128:129], WQ[:, j:j + 1], None, ALU.mult),
                             reads=['MA', 'WQ'], writes=['SM'])
                        K.op('dve', lambda e: e.scalar_tensor_tensor(SM[:, 0:1], SM[:, 6:7], -1.0, SM[:, 6:7], ALU.mult, ALU.max),
                             reads=['SM'], writes=['SM'])
                        K.op('dve', lambda e: e.tensor_scalar(SM[:, 0:1], SM[:, 0:1], 1.0, None, ALU.max), reads=['SM'], writes=['SM'])
                        K.op('dve', lambda e: e.reciprocal(SM[:, 1:2], SM[:, 0:1]), reads=['SM'], writes=['SM'])
                        K.op('dve', lambda e, j=j: e.tensor_tensor(SM[:, 2:3], SM[:, 1:2], WQ[:, j:j + 1], ALU.mult),
                             reads=['SM', 'WQ'], writes=['SM'])
                        K.op('dve', lambda e, j=j: e.scalar_tensor_tensor(HG[:], MA[:, 0:128], SM[:, 2:3], SO[:, j, :], ALU.mult, ALU.mult),
                             reads=['MA', 'SM', 'SO%d' % j], writes=['HG'])
                        K.op('act', lambda e: e.activation(JK[:], HG[:], AF.Square, accum_out=SM[:, 3:4]),
                             reads=['HG'], writes=['JK', 'SM'])
                        K.op('act', lambda e: e.activation(SM[:, 4:5], SM[:, 3:4], AF.Sqrt, bias=eps_ap, scale=1.0 / 128),
                             reads=['SM', 'cst'], writes=['SM'])
                        K.op('dve', lambda e: e.reciprocal(SM[:, 5:6], SM[:, 4:5]), reads=['SM'], writes=['SM'])
                        K.op('dve', lambda e, j=j: e.scalar_tensor_tensor(YT[:, j, 0:128], HG[:], SM[:, 5:6], ZW[:, j, :], ALU.mult, ALU.mult),
                             reads=['HG', 'SM', 'ZW%d' % j], writes=['YTm%d' % j])

                    ck('mlstm')
                    for j in range(4):
                        for half in range(2):
                            c0 = 384 + half * 128
                            K.op('pe', lambda e, j=j, half=half, c0=c0: e.transpose(TB[:, c0:c0 + 128], YT[:, j, half * 128:(half + 1) * 128], identb),
                                 reads=['YTm%d' % j if half == 0 else 'YTa%d' % j, 'cstb'], writes=['TB3%d' % half])
                            K.op('act', lambda e, j=j, half=half, c0=c0: e.copy(YTT[:, half, j * 128:(j + 1) * 128], TB[:, c0:c0 + 128]),
                                 reads=['TB3%d' % half], writes=['YTT'])
                    ccy, offy = t0 // YW, t0 % YW
                    for hf in range(2):
                        K.dma('sp', yp[l][hf][ccy].ap()[:, offy:offy + 512], YTT[:, hf, :], 'YTT', 'yp')
            except EarlyStop:
                pass
            for hf in range(2):
                for cc in range(NCY):
                    collective(yp[l][hf][cc], yg[l][hf][cc], 'yp', 'yg')
            Wf = XT[:, :, 0:256]
            Wo = Wb[:, :, 0:256]
            HTall = ['HT%d' % c for c in range(8)]
            Xs = [CA[0], CA[1]]
            Os = [SG, RS]
            K.dma('sp', Wf, wo_d[l].rearrange("c p n -> p c n"), 'd_wo', 'XT')
            K.op('dve', lambda e: e.tensor_copy(Wo, Wf), reads=['XT'], writes=['Wb'])
            for st in range(NST):
                t0 = st * 512
                ccy, offy = t0 // YW, t0 % YW
                ccx, offx = t0 // XW, t0 % XW
                for hf in range(2):
                    same = [K.b(HTall[c]) for c in range(8) if c % 2 == hf]
                    K._deps('sp', [], same)
                    K.dma('sp', HT[:].rearrange("p (r h) t -> p h r t", h=2)[:, hf, :, :],
                          yg[l][hf][ccy].ap().rearrange("(r p) t -> p r t", p=128)[:, :, offy:offy + 512], 'yg', HTall[hf])
                    for b_ in same[1:]:
                        b_.w = same[0].w
                        b_.r = []
                for m in range(2):
                    if l == 0:
                        K.dma('sp', Xs[m][:], xo_d[m, :, t0:t0 + 512], 'd_xo', 'CA%d' % m)
                    else:
                        K.dma('sp', Xs[m][:], xp[l - 1][m][ccx].ap()[:, offx:offx + 512], 'xp%d' % ((l - 1) % 2), 'CA%d' % m)
                    pbank, pname = (PA, 'PA') if m == 0 else (PB, 'PB')
                    for c in range(8):
                        K.op('pe', lambda e, c=c, m=m, pbank=pbank: e.matmul(pbank[:], Wo[:, c, m * 128:(m + 1) * 128], HT[:, c, :],
                                                                            start=(c == 0), stop=(c == 7)),
                             reads=['Wb', HTall[c]], writes=[pname])
                    oname = 'SG' if m == 0 else 'RS'
                    K.op('dve', lambda e, m=m, pbank=pbank: e.tensor_tensor(Os[m][:], pbank[:], Xs[m][:], ALU.add),
                         reads=[pname, 'CA%d' % m], writes=[oname])
                    if l == depth - 1:
                        K.dma('sp', xn_d[m, :, t0:t0 + 512], Os[m][:], oname, 'd_out')
                    else:
                        K.dma('sp', xp[l][m][ccx].ap()[:, offx:offx + 512], Os[m][:], oname, 'xp%d' % (l % 2))
            if l < depth - 1:
                for m in range(2):
                    for cc in range(NCX):
                        collective(xp[l][m][cc], xg[l][m][cc], 'xp%d' % (l % 2), 'xg')

        K.finish(['d_out'])
        nc._ninst = K.ninst
    return nc


def build_l2(S):
    NST = S // 512
    nc = bass.Bass("TRN2", target_bir_lowering=False)
    yT_d = nc.dram_tensor("yT", [8, 128, S], BF16, kind="ExternalInput").ap()
    xo_d = nc.dram_tensor("xo", [2, 128, S], F32, kind="ExternalInput").ap()
    wo_d = nc.dram_tensor("wo", [8, 128, 256], F32, kind="ExternalInput").ap()
    xn_d = nc.dram_tensor("xn", [2, 128, S], F32, kind="ExternalOutput").ap()
    with ExitStack() as es:
        def sb(name, shape, dt):
            return es.enter_context(nc.sbuf_tensor("s_" + name, shape, dt))
        K = Sync(nc, es)
        Wf = sb("Wf", [128, 8, 256], F32)
        Wo = sb("Wo", [128, 8, 256], BF16)
        Y = [sb("Y%d" % i, [128, 8, 512], BF16) for i in range(2)]
        X = [sb("X%d" % i, [128, 2, 512], F32) for i in range(2)]
        O = [sb("O%d" % i, [128, 2, 512], F32) for i in range(2)]
        P = [es.enter_context(nc.psum_tensor("p_P%d" % i, [128, 512], F32)) for i in range(4)]
        K.dma('sp', Wf[:], wo_d.rearrange("c p n -> p c n"), 'd_w', 'Wf')
        K.op('dve', lambda e: e.tensor_copy(Wo[:], Wf[:]), reads=['Wf'], writes=['Wo'])
        for st in range(NST):
            t0 = st * 512
            s = st % 2
            K.dma('sp', Y[s][:], yT_d.rearrange("c p t -> p c t")[:, :, t0:t0 + 512], 'd_y', 'Y%d' % s)
            K.dma('sp', X[s][:], xo_d.rearrange("c p t -> p c t")[:, :, t0:t0 + 512], 'd_x', 'X%d' % s)
            for m in range(2):
                pi = (st * 2 + m) % 4
                for c in range(8):
                    K.op('pe', lambda e, c=c, m=m, pi=pi, s=s: e.matmul(P[pi][:], Wo[:, c, m * 128:(m + 1) * 128], Y[s][:, c, :],
                                                                       start=(c == 0), stop=(c == 7)),
                         reads=['Wo', 'Y%d' % s], writes=['P%d' % pi])
                K.op('dve', lambda e, m=m, pi=pi, s=s: e.tensor_tensor(O[s][:, m, :], P[pi][:], X[s][:, m, :], ALU.add),
                     reads=['P%d' % pi, 'X%d' % s], writes=['O%d' % s])
            K.dma('sp', xn_d.rearrange("c p t -> p c t")[:, :, t0:t0 + 512], O[s][:], 'O%d' % s, 'd_o')
        K.finish(['d_o'])
    return nc


def _bf(a):
    return np.ascontiguousarray(a).astype(ml_dtypes.bfloat16)


def _consts_static(S):
    p = np.arange(128)
    same = (p[:, None] // 64) == (p[None, :] // 64)
    tri = (same & (p[:, None] <= p[None, :])).astype(np.float32)
    blk = same.astype(np.float32)
    sel0 = np.repeat((p < 64).astype(np.float32)[:, None], 128, 1)
    sel1 = np.repeat((p >= 64).astype(np.float32)[:, None], 128, 1)
    ident = np.eye(128, dtype=np.float32)
    t = np.arange(256)
    cm0 = np.where(t[None, :] >= p[:, None], 0.0, -NEG).astype(np.float32)
    cm1 = np.where(t[None, :] >= p[:, None] + 128, 0.0, -NEG).astype(np.float32)
    cstb = np.concatenate([ident, np.ones((128, 128), np.float32), tri, cm0, cm1], axis=1)
    s = np.arange(S)
    oh = np.zeros((33, S), np.float32)
    oh[(s // 256) % 32, s] = -NEG
    oh[32, :] = 1.0
    return dict(tri=tri, blk=blk, sel0=sel0, sel1=sel1, identf=ident, cstb=_bf(cstb), oh=_bf(oh))


def _l1_inputs(S, g, xT, layer):
    heads = (g, 7 - g)
    cs = _consts_static(S)
    w_in = layer['w_in']
    cols = []
    cols += list(range(g * 128, g * 128 + 128))
    cols += list(range(512 + g * 128, 512 + g * 128 + 128))
    cols += list(range(1024 + g * 128, 1024 + g * 128 + 128))
    cols += list(range(1536 + g * 128, 1536 + g * 128 + 128))
    cols += list(range(2048 + g * 128, 2048 + g * 128 + 128))
    cols += [2560 + g, 2564 + g]
    for h in heads:
        for base in (2568, 3080, 3592, 4104):
            cols += list(range(base + h * 64, base + h * 64 + 64))
    w = np.ascontiguousarray(w_in[:, cols]).reshape(8, 128, PROJ_W)
    cst = np.zeros((128, CST_N), np.float32)

    def put(name, arr):
        a, b = CST_OFF[name]
        cst[:, a:b] = arr

    put('normw', layer['norm_w'].reshape(8, 128).T)
    cw = np.zeros((128, 8), np.float32)
    cbv = np.zeros((128, 2), np.float32)
    for qk in range(2):
        ch = slice(qk * 512 + g * 128, qk * 512 + g * 128 + 128)
        cw[:, qk * 4:(qk + 1) * 4] = layer['conv_w'][:, ch].T
        cbv[:, qk] = layer['conv_b'][ch]
    put('convw', cw)
    put('convb', cbv)
    put('bf', np.full((128, 1), layer['b_fgate'][g], np.float32))
    put('bi', np.full((128, 1), layer['b_igate'][g], np.float32))
    put('eps', np.full((128, 1), EPS, np.float32))
    put('one', np.ones((128, 1), np.float32))
    put('mlw', np.repeat(layer['mlstm_norm_w'][g * 128:(g + 1) * 128][None, :], 128, 0))
    put('wq', np.repeat(layer['q_norm_w'][None, :], 128, 0))
    put('wk', np.repeat(layer['k_norm_w'][None, :], 128, 0))
    for n in ('identf', 'tri', 'blk', 'sel0', 'sel1'):
        put(n, cs[n])
    al = np.zeros((128, 2 * ND), np.float32)
    alr = np.zeros((2, 256), np.float32)
    sl = np.arange(128, dtype=np.float64)
    for hh, h in enumerate(heads):
        for di in range(ND):
            al[:, hh * ND + di] = (SLOPES[h] * (sl - (di - 1) * 128.0)).astype(np.float32)
        alr[hh] = -SLOPES[h] * np.arange(256)
    put('alibi', al)
    return {"xT": np.ascontiguousarray(xT.reshape(8, 128, S)), "w": w, "cst": cst, "cstb": cs['cstb'],
            "oh": cs['oh'], "alr": _bf(alr)}


_CACHE = {}


def _wo_perm(w_out, g):
    rows = []
    for r in range(4):
        rows += list(range(r * 128, r * 128 + 128))
        rows += list(range(512 + r * 64, 512 + r * 64 + 64))
        rows += list(range(512 + (7 - r) * 64, 512 + (7 - r) * 64 + 64))
    return np.ascontiguousarray(w_out[rows][:, g * 256:(g + 1) * 256]).reshape(8, 128, 256)


def kernel(x, norm_w, w_in, b_igate, b_fgate, conv_w, conv_b, mlstm_norm_w, q_norm_w, k_norm_w, w_out, depth=None):
    x = np.asarray(x, np.float32)
    Bn, S, _ = x.shape
    depth = DEPTH if depth is None else depth
    P = dict(norm_w=norm_w, w_in=w_in, b_igate=b_igate, b_fgate=b_fgate, conv_w=conv_w, conv_b=conv_b,
             mlstm_norm_w=mlstm_norm_w, q_norm_w=q_norm_w, k_norm_w=k_norm_w, w_out=w_out)
    P = {k: np.asarray(v, np.float32) for k, v in P.items()}
    key = ('fused', S, depth)
    if key not in _CACHE:
        _CACHE[key] = build_fused(S, depth)
    nc = _CACHE[key]
    in_maps = []
    for c in range(8):
        b, g = c // 4, c % 4
        xT = np.ascontiguousarray(x[b].T)
        per = [_l1_inputs(S, g, xT, {k: v[l] for k, v in P.items()}) for l in range(depth)]
        in_maps.append({
            "xT": per[0]["xT"],
            "xo": np.ascontiguousarray(xT[g * 256:(g + 1) * 256].reshape(2, 128, S)),
            "w": np.stack([p["w"] for p in per], 0),
            "cst": np.stack([p["cst"] for p in per], 0),
            "wo": np.stack([_wo_perm(P['w_out'][l], g) for l in range(depth)], 0),
            "cstb": per[0]["cstb"], "oh": per[0]["oh"], "alr": per[0]["alr"]})
    res = run_bass_kernel_spmd(nc, in_maps, core_ids=list(range(8)))
    out = np.empty((Bn, S, D_MODEL), np.float32)
    for c in range(8):
        b, g = c // 4, c % 4
        out[b, :, g * 256:(g + 1) * 256] = np.asarray(res.results[c]["xn"]).reshape(256, S).T
    return out
```

```python
import numpy as np
import ml_dtypes
from contextlib import ExitStack
import concourse.bass as bass
import concourse.mybir as mybir
from concourse.bass_utils import run_bass_kernel_spmd

F32 = mybir.dt.float32
BF16 = mybir.dt.bfloat16
AF = mybir.ActivationFunctionType
ALU = mybir.AluOpType
AX = mybir.AxisListType

D_MODEL = 1024
DEPTH = 4
AT_HEADS = 8
EPS = 1e-6
PROJ_W = 1154
NF, NA, NB_ = 256, 386, 512
CUT = 80.0
NEG = 30000.0
ND = 128
SLOPES = [2.0 ** (-8.0 * (h + 1.0) / AT_HEADS) for h in range(AT_HEADS)]


def _layout(items):
    off, o = {}, 0
    for n, w in items:
        off[n] = (o, o + w)
        o += w
    return off, o


CST_OFF, CST_N = _layout([('normw', 8), ('convw', 8), ('convb', 2), ('bf', 1), ('bi', 1), ('eps', 1), ('one', 1),
                          ('mlw', 128), ('wq', 64), ('wk', 64), ('identf', 128), ('tri', 128), ('blk', 128),
                          ('sel0', 128), ('sel1', 128), ('alibi', 2 * ND)])
CSTB_OFF, CSTB_N = _layout([('identb', 128), ('onesb', 128), ('cmml', 128), ('cm0', 256), ('cm1', 256)])


class Buf:
    __slots__ = ("name", "w", "r", "wsem", "wcnt")

    def __init__(self, name):
        self.name = name
        self.w = None
        self.r = []
        self.wsem = None
        self.wcnt = 0


class Sync:
    ROT = 30000

    def __init__(self, nc, es):
        self.nc = nc
        self.es = es
        self.engs = {'pe': nc.tensor, 'act': nc.scalar, 'dve': nc.vector, 'pool': nc.gpsimd, 'sp': nc.sync}
        self.sem = {}
        self.cnt = {}
        self.waited = {e: {} for e in self.engs}
        self.nsem = 0
        for e in self.engs:
            self.sem[e] = self.newsem('prog_' + e)
            self.cnt[e] = 0
        self.ninst = 0
        self.bufs = {}
        self.alias = {}
        self.excl = set()

    def b(self, n):
        n = self.alias.get(n, n)
        if n not in self.bufs:
            self.bufs[n] = Buf(n)
        return self.bufs[n]

    def newsem(self, name):
        self.nsem += 1
        return self.es.enter_context(self.nc.semaphore(name + '_%d' % self.nsem))

    def _emit_waits(self, e, toks):
        need = {}
        for tok in toks:
            if tok is None:
                continue
            sem, val = tok
            k = id(sem)
            if self.waited[e].get(k, 0) >= val:
                continue
            if k not in need or need[k][1] < val:
                need[k] = (sem, val)
        for k, (sem, val) in need.items():
            self.waited[e][k] = val
            self.engs[e].wait_ge(sem, val)

    def _deps(self, e, reads, writes):
        toks = []
        for b in reads:
            toks.append(b.w)
        for b in writes:
            toks.append(b.w)
            toks.extend(b.r)
        self._emit_waits(e, toks)

    def _bl(self, names):
        return [self.b(n) if isinstance(n, str) else n for n in names]

    def op(self, e, fn, reads=(), writes=()):
        reads = self._bl(reads)
        writes = self._bl(writes)
        xr = [b for b in reads if b.name in self.excl and b not in writes]
        if xr:
            writes = writes + xr
        self._deps(e, reads, writes)
        ins = fn(self.engs[e])
        if self.cnt[e] >= self.ROT:
            self.sem[e] = self.newsem('prog_' + e)
            self.cnt[e] = 0
        self.cnt[e] += 1
        ins.then_inc(self.sem[e], 1)
        tok = (self.sem[e], self.cnt[e])
        for b in reads:
            b.r.append(tok)
        for b in writes:
            b.w = tok
            b.r = []
        self.ninst += 1
        return ins

    def dma(self, q, out_ap, in_ap, src, dst, **kw):
        src, dst = self._bl([src, dst])
        self._deps(q, [src], [dst])
        if dst.wsem is None or dst.wcnt >= self.ROT:
            dst.wsem = self.newsem('w_' + dst.name)
            dst.wcnt = 0
        ins = self.engs[q].dma_start(out=out_ap, in_=in_ap, **kw)
        dst.wcnt += 16
        ins.then_inc(dst.wsem, 16)
        tok = (dst.wsem, dst.wcnt)
        src.r.append(tok)
        dst.w = tok
        dst.r = []
        self.ninst += 1
        return ins

    def finish(self, names):
        toks = []
        for b in self._bl(names):
            toks.append(b.w)
            toks.extend(b.r)
        for b in self.bufs.values():
            if b.wsem is not None:
                toks.append((b.wsem, b.wcnt))
        self._emit_waits('sp', toks)


STOP = None


class EarlyStop(Exception):
    pass


def ck(name):
    if STOP == name:
        raise EarlyStop()


CUT_SLOPE = (SLOPES[3], SLOPES[7])


def _visit_tiles(hh, qb):
    slope = CUT_SLOPE[hh]
    out = []
    for kt in range(0, 2 * qb):
        delta = 2 * qb - kt
        if slope * (delta * 128 - 127) > CUT:
            continue
        out.append(kt)
    return out


def build_fused(S, depth):
    NT = S // 128
    NST = S // 512
    YW = min(4096, S)
    XW = min(2048, S)
    NCY = S // YW
    NCX = S // XW
    groups = [[0, 1, 2, 3], [4, 5, 6, 7]]
    nc = bass.Bass("TRN2", target_bir_lowering=False)
    xT_d = nc.dram_tensor("xT", [8, 128, S], F32, kind="ExternalInput").ap()
    xo_d = nc.dram_tensor("xo", [2, 128, S], F32, kind="ExternalInput").ap()
    w_d = nc.dram_tensor("w", [depth, 8, 128, PROJ_W], F32, kind="ExternalInput").ap()
    cst_d = nc.dram_tensor("cst", [depth, 128, CST_N], F32, kind="ExternalInput").ap()
    wo_d = nc.dram_tensor("wo", [depth, 8, 128, 256], F32, kind="ExternalInput").ap()
    cstb_d = nc.dram_tensor("cstb", [128, CSTB_N], BF16, kind="ExternalInput").ap()
    oh_d = nc.dram_tensor("oh", [33, S], BF16, kind="ExternalInput").ap()
    alr_d = nc.dram_tensor("alr", [2, 256], BF16, kind="ExternalInput").ap()
    xn_d = nc.dram_tensor("xn", [2, 128, S], F32, kind="ExternalOutput").ap()
    yp1 = [[nc.dram_tensor("yp_%d_%d" % (hf, cc), [128, YW], BF16) for cc in range(NCY)] for hf in range(2)]
    yg1 = [[nc.dram_tensor("yg_%d_%d" % (hf, cc), [512, YW], BF16) for cc in range(NCY)] for hf in range(2)]
    xp2 = [[[nc.dram_tensor("xp%d_%d_%d" % (q, m, cc), [128, XW], F32) for cc in range(NCX)] for m in range(2)] for q in range(2)]
    xg1 = [[nc.dram_tensor("xg_%d_%d" % (m, cc), [512, XW], F32) for cc in range(NCX)] for m in range(2)]
    yp = [yp1] * depth
    yg = [yg1] * depth
    xp = [xp2[l % 2] for l in range(depth)]
    xg = [xg1] * depth

    with ExitStack() as es:
        def sb(name, shape, dt):
            return es.enter_context(nc.sbuf_tensor("s_" + name, shape, dt))

        def ps(name, shape, dt):
            return es.enter_context(nc.psum_tensor("p_" + name, shape, dt))

        K = Sync(nc, es)
        K.alias = {'TB0': 'TB', 'TB1': 'TB', 'TB2': 'TB', 'TB30': 'TB', 'TB31': 'TB', 'TBm0': 'TB', 'TBm1': 'TB',
                   'MSg': 'MS', 'MSs': 'MS', 'MSd0': 'MS', 'MSd1': 'MS', 'MSgate': 'MS',
                   'AS0': 'AS', 'AS1': 'PF', 'AOt0': 'AO', 'AOt1': 'AO'}
        K.excl = {'PF', 'PA', 'PB', 'TB', 'MS', 'MA', 'AS', 'AO'}
        cst = sb("cst", [128, CST_N], F32)
        cstb = sb("cstb", [128, CSTB_N], BF16)
        Wb = sb("Wb", [128, 8, PROJ_W], BF16)
        XT = sb("XT", [128, 8, 512], F32)
        SQ = sb("SQ", [128, 2, 512], BF16)
        HT = sb("HT", [128, 8, 512], BF16)
        RS = sb("RS", [128, 512], F32)
        XC = [sb("XC%d" % i, [128, 515], F32) for i in range(2)]
        CA = [sb("CA%d" % i, [128, 512], F32) for i in range(2)]
        SG = sb("SG", [128, 512], F32)
        QT = sb("QT", [128, 512], BF16)
        KT = sb("KT", [128, 512], BF16)
        VML = sb("VML", [128, 4, 129], BF16)
        SO = sb("SO", [128, 4, 128], F32)
        SZ = sb("SZ", [128, 4, 128], F32)
        ZW = sb("ZW", [128, 4, 128], F32)
        IFt = sb("IFt", [128, 2, 4], F32)
        GT = sb("GT", [128, 8, 4], F32)
        WK = sb("WK", [128, 4], F32)
        WQ = sb("WQ", [128, 4], F32)
        SC = sb("SC", [128, 8], F32)
        KK = sb("KK", [128, 128], BF16)
        PT = sb("PT", [128, 128], BF16)
        Cst = sb("Cst", [128, 129], F32)
        CP = sb("CP", [128, 129], F32)
        CB = [sb("CB%d" % i, [128, 129], BF16) for i in range(2)]
        SM = sb("SM", [128, 8], F32)
        HG = sb("HG", [128, 128], F32)
        JK = sb("JK", [128, 128], F32)
        YT = sb("YT", [128, 4, 256], BF16)
        YTT = sb("YTT", [128, 2, 512], BF16)
        SS = sb("SS", [128, 4], F32)
        QN = sb("QN", [128, 2, 2, 2, 96], BF16)
        KN = sb("KN", [128, 64], BF16)
        SZA = sb("SZA", [128, 4, 128], F32)
        KTA = [sb("KTA%d" % i, [97, S], BF16) for i in range(2)]
        VAT = [sb("VAT%d" % i, [128, NT, 65], BF16) for i in range(2)]
        KM = [sb("KM%d" % i, [64, 64], BF16) for i in range(2)]
        KMS = sb("KMS", [64, 2], F32)
        QTA = [[sb("QTA%d%d" % (i, v), [97, 256], BF16) for v in range(2)] for i in range(2)]
        G = sb("G", [128, 64], F32)
        T8 = sb("T8", [128, 8], F32)
        PTA = [sb("PTA%d" % i, [128, 256], BF16) for i in range(3)]
        OT = sb("OT", [65, 256], F32)
        AM = sb("AM", [128, 4], F32)
        PF = ps("PF", [128, 512], F32)
        PA = ps("PA", [128, 512], F32)
        PB = ps("PB", [128, 512], F32)
        TB = ps("TB", [128, 1024], BF16)
        MS = ps("MS", [128, 512], F32)
        MA = ps("MA", [128, 512], F32)
        AS = ps("AS", [128, 512], F32)
        AO = ps("AO", [128, 512], F32)

        def C(name):
            a, b_ = CST_OFF[name]
            return cst[:, a:b_]

        def CBV(name):
            a, b_ = CSTB_OFF[name]
            return cstb[:, a:b_]

        identf = C('identf')
        identb = CBV('identb')
        onesb = CBV('onesb')
        eps_ap = C('eps')[:, 0:1]
        Wst = XT[:].rearrange("p c t -> p (c t)")[:, 0:PROJ_W]

        K.dma('sp', cstb[:], cstb_d[:, :], 'd_cstb', 'cstb')
        for i in range(2):
            K.dma('sp', KTA[i][64:97, :], oh_d[:, :], 'd_oh', 'KTAc%d' % i)
            for v in range(2):
                K.dma('sp', QTA[i][v][96:97, :], alr_d[i:i + 1, :], 'd_alr', 'QTAc%d%d' % (i, v))
            K.op('pool', lambda e, i=i: e.memset(VAT[i][:], 1.0), writes=['VATones%d' % i])
        K.op('pool', lambda e: e.memset(VML[:], 1.0), writes=['VMLones'])
        for i in range(2):
            for kt in range(NT):
                K.b('VAT%d_%d' % (i, kt)).w = K.b('VATones%d' % i).w
        for j in range(4):
            K.b('VML%d' % j).w = K.b('VMLones').w

        def collective(in_t, out_t, src, dst):
            src, dst = K._bl([src, dst])
            K._deps('pool', [src], [dst])
            if dst.wsem is None:
                dst.wsem = K.newsem('cc_' + dst.name)
                dst.wcnt = 0
            ins = nc.gpsimd.collective_compute("AllGather", ALU.bypass, replica_groups=groups,
                                               ins=[in_t.ap().opt()], outs=[out_t.ap().opt()])
            dst.wcnt += 1
            ins.then_inc(dst.wsem)
            tok = (dst.wsem, dst.wcnt)
            src.r.append(tok)
            dst.w = tok
            dst.r = []

        QSCALE = 128.0 ** -0.5

        def attention(st, qbl):
            qb = st * 2 + qbl
            for hh in range(2):
                visit = _visit_tiles(hh, qb)
                tiles = visit + [2 * qb, 2 * qb + 1]
                variants = sorted(set((kt // 2) // 32 for kt in tiles))
                if qb >= 4:
                    ncol = max(8, qb)
                    for sub in range(2):
                        qnb = 'QN%d%d' % (hh, sub)
                        K.op('pe', lambda e, sub=sub: e.matmul(MS[:, 416:416 + qb], QTA[hh][0][0:64, sub * 128:(sub + 1) * 128],
                                                               KM[hh][:, 0:qb], start=True, stop=True),
                             reads=['QTAq%d0' % hh] + ['KM%d_%d' % (hh, bl) for bl in range(qb)], writes=['MSgate'])
                        K.op('dve', lambda e: e.tensor_copy(G[:, 0:qb], MS[:, 416:416 + qb]), reads=['MSgate'], writes=['G'])
                        K.op('dve', lambda e: e.max(T8[:], G[:, 0:ncol]), reads=['G'], writes=['T8'])
                        for v in variants:
                            K.op('dve', lambda e, sub=sub, v=v: e.tensor_scalar(QN[:, hh, sub, v, 64:96], G[:, 32 * v:32 * v + 32],
                                                                               T8[:, 2:3], None, ALU.is_lt),
                                 reads=['G', 'T8'], writes=[qnb])
                        vo, co = qb // 32, qb % 32
                        K.op('dve', lambda e, sub=sub: e.memset(QN[:, hh, sub, vo, 64 + co:65 + co], 0.0), writes=[qnb])
                        for v in variants:
                            K.op('pe', lambda e, sub=sub, v=v: e.transpose(TB[0:96, 512 + v * 128:640 + v * 128], QN[:, hh, sub, v, :], identb),
                                 reads=[qnb, 'cstb'], writes=['TBm%d' % v])
                            K.op('dve', lambda e, sub=sub, v=v: e.tensor_copy(QTA[hh][v][64:96, sub * 128:(sub + 1) * 128],
                                                                             TB[64:96, 512 + v * 128:640 + v * 128]),
                                 reads=['TBm%d' % v], writes=['QTAm%d%d' % (hh, v)])
                nt_ = len(tiles)
                ASB = [(AS, 'AS0'), (PF, 'AS1')]

                def emit_score(idx):
                    kt = tiles[idx]
                    v = (kt // 2) // 32
                    bank, asb = ASB[idx % 2]
                    own = kt >= 2 * qb
                    K.op('pe', lambda e: e.matmul(bank[:, 0:256], KTA[hh][0:97, kt * 128:(kt + 1) * 128], QTA[hh][v][0:97, 0:256],
                                                  start=True, stop=not own),
                         reads=['KTA%d_%d' % (hh, kt), 'KTAc%d' % hh, 'QTAq%d%d' % (hh, v), 'QTAm%d%d' % (hh, v), 'QTAc%d%d' % (hh, v)],
                         writes=[asb])
                    if own:
                        cmn = 'cm0' if kt == 2 * qb else 'cm1'
                        K.op('pe', lambda e: e.matmul(bank[:, 0:256], identb, CBV(cmn), start=False, stop=True),
                             reads=['cstb'], writes=[asb])

                emit_score(0)
                for idx, kt in enumerate(tiles):
                    bank, asb = ASB[idx % 2]
                    delta = 2 * qb - kt
                    col = CST_OFF['alibi'][0] + hh * ND + delta + 1
                    pi = idx % 3
                    K.op('act', lambda e, bank=bank, col=col, pi=pi: e.activation(PTA[pi][:], bank[:, 0:256], AF.Exp,
                                                                                  bias=cst[:, col:col + 1]),
                         reads=[asb, 'cst'], writes=['PTA%d' % pi])
                    if idx + 1 < nt_:
                        emit_score(idx + 1)
                    K.op('pe', lambda e, kt=kt, pi=pi, idx=idx: e.matmul(AO[0:65, 0:256], VAT[hh][:, kt, :], PTA[pi][:],
                                                                        start=(idx == 0), stop=(idx == nt_ - 1)),
                         reads=['VAT%d_%d' % (hh, kt), 'PTA%d' % pi], writes=['AO'])
                K.op('act', lambda e: e.copy(OT[:], AO[0:65, 0:256]), reads=['AO'], writes=['OT'])
                for sub in range(2):
                    j = 2 * qbl + sub
                    c0 = 256 + sub * 128
                    K.op('pe', lambda e, sub=sub, c0=c0: e.transpose(AO[:, c0:c0 + 65], OT[:, sub * 128:(sub + 1) * 128], identf[0:65, 0:65]),
                         reads=['OT', 'cst'], writes=['AOt%d' % sub])
                    K.op('dve', lambda e, sub=sub, c0=c0: e.reciprocal(AM[:, sub:sub + 1], AO[:, c0 + 64:c0 + 65]),
                         reads=['AOt%d' % sub], writes=['AM%d' % sub])
                    K.op('dve', lambda e, sub=sub, c0=c0, j=j: e.scalar_tensor_tensor(
                        YT[:, j, 128 + hh * 64:128 + (hh + 1) * 64], AO[:, c0:c0 + 64], AM[:, sub:sub + 1],
                        SZA[:, j, hh * 64:(hh + 1) * 64], ALU.mult, ALU.mult),
                         reads=['AOt%d' % sub, 'AM%d' % sub, 'SZA%d' % j], writes=['YTa%d' % j])

        for l in range(depth):
            K.dma('sp', cst[:], cst_d[l, :, :], 'd_cst', 'cst')
            for c in range(8):
                K.dma('sp', Wst, w_d[l, c, :, :], 'd_w', 'XT')
                K.op('dve', lambda e, c=c: e.tensor_scalar(Wb[:, c, :], Wst, C('normw')[:, c:c + 1], None, ALU.mult),
                     reads=['XT', 'cst'], writes=['Wb'])
            for i in range(2):
                for v in range(2):
                    K.op('pool', lambda e, i=i, v=v: e.memset(QTA[i][v][64:96, :], 0.0), writes=['QTAm%d%d' % (i, v)])
                K.op('pool', lambda e, i=i: e.memset(XC[i][:, 0:3], 0.0), writes=['XCh%d' % i])
            K.op('pool', lambda e: e.memset(Cst[:], 0.0), writes=['Cst'])
            K.op('pool', lambda e: e.memset(G[:], -1e30), writes=['G'])
            K.op('pool', lambda e: e.memset(QN[:], 0.0), writes=['QN00', 'QN01', 'QN10', 'QN11'])

            try:
                ck('setup')
                for st in range(NST):
                    t0 = st * 512
                    if l == 0:
                        K.dma('sp', XT[:], xT_d.rearrange("c p t -> p c t")[:, :, t0:t0 + 512], 'd_x', 'XT')
                    else:
                        ccx, offx = t0 // XW, t0 % XW
                        for m in range(2):
                            K.dma('sp', XT[:].rearrange("p (r h) t -> p h r t", h=2)[:, m, :, :],
                                  xg[l - 1][m][ccx].ap().rearrange("(r p) t -> p r t", p=128)[:, :, offx:offx + 512], 'xg_%d' % ccx, 'XT')
                    for c in range(8):
                        sl = c % 2
                        K.op('act', lambda e, c=c, sl=sl: e.activation(SQ[:, sl, :], XT[:, c, :], AF.Square), reads=['XT'], writes=['SQ%d' % sl])
                        K.op('pe', lambda e, c=c, sl=sl: e.matmul(PF[:], onesb, SQ[:, sl, :], start=(c == 0), stop=(c == 7)),
                             reads=['SQ%d' % sl, 'cstb'], writes=['PF'])
                    K.op('act', lambda e: e.activation(RS[:], PF[:], AF.Sqrt, bias=eps_ap, scale=1.0 / D_MODEL),
                         reads=['PF', 'cst'], writes=['RS'])
                    K.op('dve', lambda e: e.reciprocal(RS[:], RS[:]), reads=['RS'], writes=['RS'])
                    for c in range(8):
                        eng = 'dve' if c % 2 == 0 else 'pool'
                        K.op(eng, lambda e, c=c: e.tensor_tensor(HT[:, c, :], XT[:, c, :], RS[:], ALU.mult),
                             reads=['XT', 'RS'], writes=['HT%d' % c])
                    HTb = ['HT%d' % c for c in range(8)]
                    ck('norm')
                    cw = C('convw')
                    cb = C('convb')
                    for qk in range(2):
                        for c in range(8):
                            K.op('pe', lambda e, c=c, qk=qk: e.matmul(PF[:], Wb[:, c, qk * 128:(qk + 1) * 128], HT[:, c, :],
                                                                     start=(c == 0), stop=(c == 7)),
                                 reads=['Wb', HTb[c]], writes=['PF'])
                        K.op('act', lambda e, qk=qk: e.copy(XC[qk][:, 3:515], PF[:]), reads=['PF'], writes=['XCb%d' % qk])
                        K.op('dve', lambda e, qk=qk: e.tensor_scalar(CA[qk][:], XC[qk][:, 3:515], cw[:, qk * 4 + 3:qk * 4 + 4],
                                                                     cb[:, qk:qk + 1], ALU.mult, ALU.add),
                             reads=['XCb%d' % qk, 'cst'], writes=['CA%d' % qk])
                        for k in (2, 1, 0):
                            K.op('dve', lambda e, qk=qk, k=k: e.scalar_tensor_tensor(CA[qk][:], XC[qk][:, k:k + 512],
                                                                                    cw[:, qk * 4 + k:qk * 4 + k + 1], CA[qk][:],
                                                                                    ALU.mult, ALU.add),
                                 reads=['XCb%d' % qk, 'XCh%d' % qk, 'cst', 'CA%d' % qk], writes=['CA%d' % qk])
                        K.op('pool', lambda e, qk=qk: e.tensor_copy(XC[qk][:, 0:3], XC[qk][:, 512:515]),
                             reads=['XCb%d' % qk], writes=['XCh%d' % qk])
                        if qk == 0:
                            K.op('act', lambda e: e.activation(SG[:], CA[0][:], AF.Sigmoid), reads=['CA0'], writes=['SG'])
                            K.op('dve', lambda e: e.scalar_tensor_tensor(QT[:], CA[0][:], QSCALE, SG[:], ALU.mult, ALU.mult),
                                 reads=['CA0', 'SG'], writes=['QT'])
                        else:
                            K.op('act', lambda e: e.activation(KT[:], CA[1][:], AF.Silu), reads=['CA1'], writes=['KT'])
                    ck('conv')
                    for j in range(4):
                        tg = st * 4 + j
                        for c in range(8):
                            K.op('pe', lambda e, c=c, j=j: e.matmul(PA[:, 0:NA], HT[:, c, j * 128:(j + 1) * 128],
                                                                   Wb[:, c, NF:NF + NA], start=(c == 0), stop=(c == 7)),
                                 reads=['Wb', HTb[c]], writes=['PA'])
                        for c in range(8):
                            K.op('pe', lambda e, c=c, j=j: e.matmul(PB[:, 0:NB_], HT[:, c, j * 128:(j + 1) * 128],
                                                                   Wb[:, c, NF + NA:PROJ_W], start=(c == 0), stop=(c == 7)),
                                 reads=['Wb', HTb[c]], writes=['PB'])
                        ck('pA')
                        K.op('act', lambda e, j=j: e.copy(VML[:, j, 0:128], PA[:, 0:128]), reads=['PA'], writes=['VML%d' % j])
                        K.op('act', lambda e, j=j: e.activation(SO[:, j, :], PA[:, 128:256], AF.Sigmoid), reads=['PA'], writes=['SO%d' % j])
                        K.op('act', lambda e, j=j: e.activation(SZ[:, j, :], PA[:, 256:384], AF.Silu), reads=['PA'], writes=['SZ%d' % j])
                        K.op('pool', lambda e, j=j: e.tensor_tensor(ZW[:, j, :], SZ[:, j, :], C('mlw'), ALU.mult),
                             reads=['SZ%d' % j, 'cst'], writes=['ZW%d' % j])
                        K.op('dve', lambda e, j=j: e.tensor_copy(IFt[:, :, j], PA[:, 384:386]), reads=['PA'], writes=['IFt'])
                        ck('evA')
                        sub = j % 2
                        for hh in range(2):
                            cb0 = hh * 256
                            qnb = 'QN%d%d' % (hh, sub)
                            K.op('act', lambda e, cb0=cb0: e.activation(JK[:, 0:64], PB[:, cb0:cb0 + 64], AF.Square, accum_out=SS[:, 0:1]),
                                 reads=['PB'], writes=['JK', 'SS'])
                            K.op('act', lambda e, cb0=cb0: e.activation(JK[:, 64:128], PB[:, cb0 + 64:cb0 + 128], AF.Square, accum_out=SS[:, 1:2]),
                                 reads=['PB'], writes=['JK', 'SS'])
                            K.op('act', lambda e: e.activation(SS[:, 2:4], SS[:, 0:2], AF.Sqrt, bias=eps_ap, scale=1.0 / 64),
                                 reads=['SS', 'cst'], writes=['SS'])
                            K.op('dve', lambda e: e.reciprocal(SS[:, 2:4], SS[:, 2:4]), reads=['SS'], writes=['SS'])
                            K.op('dve', lambda e: e.tensor_scalar(SS[:, 2:3], SS[:, 2:3], 0.125, None, ALU.mult), reads=['SS'], writes=['SS'])
                            ck('evB1')
                            K.op('dve', lambda e, cb0=cb0, hh=hh, sub=sub: e.scalar_tensor_tensor(
                                QN[:, hh, sub, 0, 0:64], PB[:, cb0:cb0 + 64], SS[:, 2:3], C('wq'), ALU.mult, ALU.mult),
                                 reads=['PB', 'SS', 'cst'], writes=[qnb])
                            K.op('dve', lambda e, cb0=cb0: e.scalar_tensor_tensor(
                                KN[:], PB[:, cb0 + 64:cb0 + 128], SS[:, 3:4], C('wk'), ALU.mult, ALU.mult),
                                 reads=['PB', 'SS', 'cst'], writes=['KN'])
                            K.op('act', lambda e, cb0=cb0, hh=hh, tg=tg: e.copy(VAT[hh][:, tg, 0:64], PB[:, cb0 + 128:cb0 + 192]),
                                 reads=['PB'], writes=['VAT%d_%d' % (hh, tg)])
                            K.op('act', lambda e, cb0=cb0, hh=hh, j=j: e.activation(SZA[:, j, hh * 64:(hh + 1) * 64],
                                                                                    PB[:, cb0 + 192:cb0 + 256], AF.Silu),
                                 reads=['PB'], writes=['SZA%d' % j])
                            ck('evB2')
                            K.op('pe', lambda e, hh=hh, sub=sub: e.transpose(TB[0:64, 0:128], QN[:, hh, sub, 0, 0:64], identb),
                                 reads=[qnb, 'cstb'], writes=['TB0'])
                            ck('t1')
                            K.op('act', lambda e, hh=hh, sub=sub: e.copy(QTA[hh][0][0:64, sub * 128:(sub + 1) * 128], TB[0:64, 0:128]),
                                 reads=['TB0'], writes=['QTAq%d0' % hh])
                            ck('t2')
                            K.op('pool', lambda e, hh=hh, sub=sub: e.tensor_copy(QTA[hh][1][0:64, sub * 128:(sub + 1) * 128],
                                                                                  QTA[hh][0][0:64, sub * 128:(sub + 1) * 128]),
                                 reads=['QTAq%d0' % hh], writes=['QTAq%d1' % hh])
                            ck('t3')
                            K.op('pe', lambda e: e.transpose(TB[0:64, 128:256], KN[:], identb), reads=['KN', 'cstb'], writes=['TB1'])
                            ck('t4')
                            K.op('dve', lambda e, hh=hh, tg=tg: e.tensor_copy(KTA[hh][0:64, tg * 128:(tg + 1) * 128], TB[0:64, 128:256]),
                                 reads=['TB1'], writes=['KTA%d_%d' % (hh, tg)])
                            ck('evB3')
                            if sub == 1:
                                blk = tg // 2
                                K.op('dve', lambda e, hh=hh, blk=blk: e.reduce_sum(KMS[:, hh:hh + 1], KTA[hh][0:64, blk * 256:(blk + 1) * 256], AX.X),
                                     reads=['KTA%d_%d' % (hh, tg - 1), 'KTA%d_%d' % (hh, tg)], writes=['KMS%d' % hh])
                                K.op('act', lambda e, hh=hh, blk=blk: e.mul(KM[hh][:, blk:blk + 1], KMS[:, hh:hh + 1], 1.0 / 256),
                                     reads=['KMS%d' % hh], writes=['KM%d_%d' % (hh, blk)])
                        if sub == 1:
                            ck('proj')
                            attention(st, j // 2)
                            ck('attn')

                    K.op('dve', lambda e: e.tensor_scalar(GT[:, 0, :], IFt[:, 1, :], C('bf')[:, 0:1], None, ALU.add),
                         reads=['IFt', 'cst'], writes=['GT'])
                    K.op('act', lambda e: e.activation(GT[:, 1, :], GT[:, 0, :], AF.Exp, scale=-1.0), reads=['GT'], writes=['GT'])
                    K.op('act', lambda e: e.activation(GT[:, 2, :], GT[:, 1, :], AF.Ln, bias=C('one')[:, 0:1]), reads=['GT', 'cst'],
                         writes=['GT'])
                    for i, nm in enumerate(('tri', 'blk', 'sel0', 'sel1')):
                        K.op('pe', lambda e, i=i, nm=nm: e.matmul(MS[:, 392 + 4 * i:396 + 4 * i], C(nm), GT[:, 2, :], start=True, stop=True),
                             reads=['GT', 'cst'], writes=['MSg'])
                    K.op('dve', lambda e: e.tensor_tensor(GT[:, 3, :], IFt[:, 0, :], MS[:, 392:396], ALU.add),
                         reads=['IFt', 'MSg'], writes=['GT'])
                    K.op('dve', lambda e: e.tensor_tensor(GT[:, 3, :], GT[:, 3, :], MS[:, 396:400], ALU.subtract),
                         reads=['MSg', 'GT'], writes=['GT'])
                    K.op('act', lambda e: e.activation(WK[:], GT[:, 3, :], AF.Exp, bias=C('bi')[:, 0:1]), reads=['GT', 'cst'], writes=['WK'])
                    K.op('dve', lambda e: e.tensor_copy(GT[:, 4, :], MS[:, 396:400]), reads=['MSg'], writes=['GT'])
                    K.op('dve', lambda e: e.tensor_tensor(GT[:, 4, :], GT[:, 4, :], MS[:, 392:396], ALU.subtract),
                         reads=['MSg', 'GT'], writes=['GT'])
                    K.op('act', lambda e: e.activation(WQ[:], GT[:, 4, :], AF.Exp), reads=['GT'], writes=['WQ'])
                    K.op('act', lambda e: e.activation(SC[:].rearrange("p (j h) -> p h j", h=2),
                                                       MS[:, 400:408].rearrange("p (h j) -> p h j", h=2), AF.Exp, scale=-1.0),
                         reads=['MSg'], writes=['SC'])

                    ck('gates')
                    for j in range(4):
                        K.op('pe', lambda e, j=j: e.transpose(TB[:, 256:384], KT[:, j * 128:(j + 1) * 128], identb),
                             reads=['KT', 'cstb'], writes=['TB2'])
                        K.op('dve', lambda e, j=j: e.tensor_scalar(KK[:], TB[:, 256:384], WK[:, j:j + 1], None, ALU.mult),
                             reads=['TB2', 'WK'], writes=['KK'])
                        K.op('pe', lambda e, j=j: e.matmul(MS[:, 0:128], KT[:, j * 128:(j + 1) * 128], QT[:, j * 128:(j + 1) * 128],
                                                           start=True, stop=True),
                             reads=['KT', 'QT'], writes=['MSs'])
                        K.op('dve', lambda e, j=j: e.scalar_tensor_tensor(PT[:], MS[:, 0:128], WK[:, j:j + 1], CBV('cmml'), ALU.mult, ALU.mult),
                             reads=['MSs', 'WK', 'cstb'], writes=['PT'])
                        K.op('pe', lambda e, j=j: e.matmul(MA[:, 0:129], PT[:], VML[:, j, :], start=True, stop=False),
                             reads=['PT', 'VML%d' % j], writes=['MA'])
                        for h in range(2):
                            ci = 2 * j + h
                            lo, hi = h * 64, (h + 1) * 64
                            d0 = 128 + 130 * h
                            K.op('dve', lambda e, ci=ci: e.tensor_scalar(CP[:], Cst[:], SC[:, ci:ci + 1], None, ALU.mult),
                                 reads=['Cst', 'SC'], writes=['CP'])
                            K.op('act', lambda e, h=h: e.copy(CB[h][:], CP[:]), reads=['CP'], writes=['CB%d' % h])
                            K.op('pe', lambda e, j=j, h=h, lo=lo, hi=hi: e.matmul(MA[lo:hi, 0:129], QT[:, j * 128 + lo:j * 128 + hi], CB[h][:],
                                                                               start=False, stop=True),
                                 reads=['QT', 'CB%d' % h], writes=['MA'])
                            K.op('pe', lambda e, j=j, lo=lo, hi=hi, d0=d0: e.matmul(MS[:, d0:d0 + 129], KK[lo:hi, :], VML[lo:hi, j, :],
                                                                                 start=True, stop=True),
                                 reads=['KK', 'VML%d' % j], writes=['MSd%d' % h])
                            K.op('dve', lambda e, d0=d0: e.tensor_tensor(Cst[:], CP[:], MS[:, d0:d0 + 129], ALU.add),
                                 reads=['CP', 'MSd%d' % h], writes=['Cst'])
                        K.op('dve', lambda e, j=j: e.tensor_scalar(SM[:, 6:7], MA[:, 128:129], WQ[:, j:j + 1], None, ALU.mult),
                             reads=['MA', 'WQ'], writes=['SM'])
                        K.op('dve', lambda e: e.scalar_tensor_tensor(SM[:, 0:1], SM[:, 6:7], -1.0, SM[:, 6:7], ALU.mult, ALU.max),
                             reads=['SM'], writes=['SM'])
                        K.op('dve', lambda e: e.tensor_scalar(SM[:, 0:1], SM[:, 0:1], 1.0, None, ALU.max), reads=['SM'], writes=['SM'])
                        K.op('dve', lambda e: e.reciprocal(SM[:, 1:2], SM[:, 0:1]), reads=['SM'], writes=['SM'])
                        K.op('dve', lambda e, j=j: e.tensor_tensor(SM[:, 2:3], SM[:, 1:2], WQ[:, j:j + 1], ALU.mult),
                             reads=['SM', 'WQ'], writes=['SM'])
                        K.op('dve', lambda e, j=j: e.scalar_tensor_tensor(HG[:], MA[:, 0:128], SM[:, 2:3], SO[:, j, :], ALU.mult, ALU.mult),
                             reads=['MA', 'SM', 'SO%d' % j], writes=['HG'])
                        K.op('act', lambda e: e.activation(JK[:], HG[:], AF.Square, accum_out=SM[:, 3:4]),
                             reads=['HG'], writes=['JK', 'SM'])
                        K.op('act', lambda e: e.activation(SM[:, 4:5], SM[:, 3:4], AF.Sqrt, bias=eps_ap, scale=1.0 / 128),
                             reads=['SM', 'cst'], writes=['SM'])
                        K.op('dve', lambda e: e.reciprocal(SM[:, 5:6], SM[:, 4:5]), reads=['SM'], writes=['SM'])
                        K.op('dve', lambda e, j=j: e.scalar_tensor_tensor(YT[:, j, 0:128], HG[:], SM[:, 5:6], ZW[:, j, :], ALU.mult, ALU.mult),
                             reads=['HG', 'SM', 'ZW%d' % j], writes=['YTm%d' % j])

                    ck('mlstm')
                    for j in range(4):
                        for half in range(2):
                            c0 = 384 + half * 128
                            K.op('pe', lambda e, j=j, half=half, c0=c0: e.transpose(TB[:, c0:c0 + 128], YT[:, j, half * 128:(half + 1) * 128], identb),
                                 reads=['YTm%d' % j if half == 0 else 'YTa%d' % j, 'cstb'], writes=['TB3%d' % half])
                            K.op('act', lambda e, j=j, half=half, c0=c0: e.copy(YTT[:, half, j * 128:(j + 1) * 128], TB[:, c0:c0 + 128]),
                                 reads=['TB3%d' % half], writes=['YTT'])
                    ccy, offy = t0 // YW, t0 % YW
                    for hf in range(2):
                        K.dma('sp', yp[l][hf][ccy].ap()[:, offy:offy + 512], YTT[:, hf, :], 'YTT', 'yp')
            except EarlyStop:
                pass
            for cc in range(NCY):
                for hf in range(2):
                    collective(yp[l][hf][cc], yg[l][hf][cc], 'yp', 'yg_%d' % cc)
            Wf = XT[:, :, 0:256]
            Wo = Wb[:, :, 0:256]
            HTall = ['HT%d' % c for c in range(8)]
            Xs = [CA[0], CA[1]]
            Os = [SG, RS]
            K.dma('sp', Wf, wo_d[l].rearrange("c p n -> p c n"), 'd_wo', 'XT')
            K.op('dve', lambda e: e.tensor_copy(Wo, Wf), reads=['XT'], writes=['Wb'])
            for st in range(NST):
                t0 = st * 512
                ccy, offy = t0 // YW, t0 % YW
                ccx, offx = t0 // XW, t0 % XW
                for hf in range(2):
                    same = [K.b(HTall[c]) for c in range(8) if c % 2 == hf]
                    K._deps('sp', [], same)
                    K.dma('sp', HT[:].rearrange("p (r h) t -> p h r t", h=2)[:, hf, :, :],
                          yg[l][hf][ccy].ap().rearrange("(r p) t -> p r t", p=128)[:, :, offy:offy + 512], 'yg_%d' % ccy, HTall[hf])
                    for b_ in same[1:]:
                        b_.w = same[0].w
                        b_.r = []
                for m in range(2):
                    if l == 0:
                        K.dma('sp', Xs[m][:], xo_d[m, :, t0:t0 + 512], 'd_xo', 'CA%d' % m)
                    else:
                        K.dma('sp', Xs[m][:], xp[l - 1][m][ccx].ap()[:, offx:offx + 512], 'xp%d' % ((l - 1) % 2), 'CA%d' % m)
                    pbank, pname = (PA, 'PA') if m == 0 else (PB, 'PB')
                    for c in range(8):
                        K.op('pe', lambda e, c=c, m=m, pbank=pbank: e.matmul(pbank[:], Wo[:, c, m * 128:(m + 1) * 128], HT[:, c, :],
                                                                            start=(c == 0), stop=(c == 7)),
                             reads=['Wb', HTall[c]], writes=[pname])
                    oname = 'SG' if m == 0 else 'RS'
                    K.op('dve', lambda e, m=m, pbank=pbank: e.tensor_tensor(Os[m][:], pbank[:], Xs[m][:], ALU.add),
                         reads=[pname, 'CA%d' % m], writes=[oname])
                    if l == depth - 1:
                        K.dma('sp', xn_d[m, :, t0:t0 + 512], Os[m][:], oname, 'd_out')
                    else:
                        K.dma('sp', xp[l][m][ccx].ap()[:, offx:offx + 512], Os[m][:], oname, 'xp%d' % (l % 2))
                if l < depth - 1 and (t0 + 512) % XW == 0:
                    for m in range(2):
                        collective(xp[l][m][ccx], xg[l][m][ccx], 'xp%d' % (l % 2), 'xg_%d' % ccx)

        K.finish(['d_out'])
        nc._ninst = K.ninst
    return nc


def build_l2(S):
    NST = S // 512
    nc = bass.Bass("TRN2", target_bir_lowering=False)
    yT_d = nc.dram_tensor("yT", [8, 128, S], BF16, kind="ExternalInput").ap()
    xo_d = nc.dram_tensor("xo", [2, 128, S], F32, kind="ExternalInput").ap()
    wo_d = nc.dram_tensor("wo", [8, 128, 256], F32, kind="ExternalInput").ap()
    xn_d = nc.dram_tensor("xn", [2, 128, S], F32, kind="ExternalOutput").ap()
    with ExitStack() as es:
        def sb(name, shape, dt):
            return es.enter_context(nc.sbuf_tensor("s_" + name, shape, dt))
        K = Sync(nc, es)
        Wf = sb("Wf", [128, 8, 256], F32)
        Wo = sb("Wo", [128, 8, 256], BF16)
        Y = [sb("Y%d" % i, [128, 8, 512], BF16) for i in range(2)]
        X = [sb("X%d" % i, [128, 2, 512], F32) for i in range(2)]
        O = [sb("O%d" % i, [128, 2, 512], F32) for i in range(2)]
        P = [es.enter_context(nc.psum_tensor("p_P%d" % i, [128, 512], F32)) for i in range(4)]
        K.dma('sp', Wf[:], wo_d.rearrange("c p n -> p c n"), 'd_w', 'Wf')
        K.op('dve', lambda e: e.tensor_copy(Wo[:], Wf[:]), reads=['Wf'], writes=['Wo'])
        for st in range(NST):
            t0 = st * 512
            s = st % 2
            K.dma('sp', Y[s][:], yT_d.rearrange("c p t -> p c t")[:, :, t0:t0 + 512], 'd_y', 'Y%d' % s)
            K.dma('sp', X[s][:], xo_d.rearrange("c p t -> p c t")[:, :, t0:t0 + 512], 'd_x', 'X%d' % s)
            for m in range(2):
                pi = (st * 2 + m) % 4
                for c in range(8):
                    K.op('pe', lambda e, c=c, m=m, pi=pi, s=s: e.matmul(P[pi][:], Wo[:, c, m * 128:(m + 1) * 128], Y[s][:, c, :],
                                                                       start=(c == 0), stop=(c == 7)),
                         reads=['Wo', 'Y%d' % s], writes=['P%d' % pi])
                K.op('dve', lambda e, m=m, pi=pi, s=s: e.tensor_tensor(O[s][:, m, :], P[pi][:], X[s][:, m, :], ALU.add),
                     reads=['P%d' % pi, 'X%d' % s], writes=['O%d' % s])
            K.dma('sp', xn_d.rearrange("c p t -> p c t")[:, :, t0:t0 + 512], O[s][:], 'O%d' % s, 'd_o')
        K.finish(['d_o'])
    return nc


def _bf(a):
    return np.ascontiguousarray(a).astype(ml_dtypes.bfloat16)


def _consts_static(S):
    p = np.arange(128)
    same = (p[:, None] // 64) == (p[None, :] // 64)
    tri = (same & (p[:, None] <= p[None, :])).astype(np.float32)
    blk = same.astype(np.float32)
    sel0 = np.repeat((p < 64).astype(np.float32)[:, None], 128, 1)
    sel1 = np.repeat((p >= 64).astype(np.float32)[:, None], 128, 1)
    ident = np.eye(128, dtype=np.float32)
    t = np.arange(256)
    cm0 = np.where(t[None, :] >= p[:, None], 0.0, -NEG).astype(np.float32)
    cm1 = np.where(t[None, :] >= p[:, None] + 128, 0.0, -NEG).astype(np.float32)
    cstb = np.concatenate([ident, np.ones((128, 128), np.float32), tri, cm0, cm1], axis=1)
    s = np.arange(S)
    oh = np.zeros((33, S), np.float32)
    oh[(s // 256) % 32, s] = -NEG
    oh[32, :] = 1.0
    return dict(tri=tri, blk=blk, sel0=sel0, sel1=sel1, identf=ident, cstb=_bf(cstb), oh=_bf(oh))


def _l1_inputs(S, g, xT, layer):
    heads = (g, 7 - g)
    cs = _consts_static(S)
    w_in = layer['w_in']
    cols = []
    cols += list(range(g * 128, g * 128 + 128))
    cols += list(range(512 + g * 128, 512 + g * 128 + 128))
    cols += list(range(1024 + g * 128, 1024 + g * 128 + 128))
    cols += list(range(1536 + g * 128, 1536 + g * 128 + 128))
    cols += list(range(2048 + g * 128, 2048 + g * 128 + 128))
    cols += [2560 + g, 2564 + g]
    for h in heads:
        for base in (2568, 3080, 3592, 4104):
            cols += list(range(base + h * 64, base + h * 64 + 64))
    w = np.ascontiguousarray(w_in[:, cols]).reshape(8, 128, PROJ_W)
    cst = np.zeros((128, CST_N), np.float32)

    def put(name, arr):
        a, b = CST_OFF[name]
        cst[:, a:b] = arr

    put('normw', layer['norm_w'].reshape(8, 128).T)
    cw = np.zeros((128, 8), np.float32)
    cbv = np.zeros((128, 2), np.float32)
    for qk in range(2):
        ch = slice(qk * 512 + g * 128, qk * 512 + g * 128 + 128)
        cw[:, qk * 4:(qk + 1) * 4] = layer['conv_w'][:, ch].T
        cbv[:, qk] = layer['conv_b'][ch]
    put('convw', cw)
    put('convb', cbv)
    put('bf', np.full((128, 1), layer['b_fgate'][g], np.float32))
    put('bi', np.full((128, 1), layer['b_igate'][g], np.float32))
    put('eps', np.full((128, 1), EPS, np.float32))
    put('one', np.ones((128, 1), np.float32))
    put('mlw', np.repeat(layer['mlstm_norm_w'][g * 128:(g + 1) * 128][None, :], 128, 0))
    put('wq', np.repeat(layer['q_norm_w'][None, :], 128, 0))
    put('wk', np.repeat(layer['k_norm_w'][None, :], 128, 0))
    for n in ('identf', 'tri', 'blk', 'sel0', 'sel1'):
        put(n, cs[n])
    al = np.zeros((128, 2 * ND), np.float32)
    alr = np.zeros((2, 256), np.float32)
    sl = np.arange(128, dtype=np.float64)
    for hh, h in enumerate(heads):
        for di in range(ND):
            al[:, hh * ND + di] = (SLOPES[h] * (sl - (di - 1) * 128.0)).astype(np.float32)
        alr[hh] = -SLOPES[h] * np.arange(256)
    put('alibi', al)
    return {"xT": np.ascontiguousarray(xT.reshape(8, 128, S)), "w": w, "cst": cst, "cstb": cs['cstb'],
            "oh": cs['oh'], "alr": _bf(alr)}


_CACHE = {}


def _wo_perm(w_out, g):
    rows = []
    for r in range(4):
        rows += list(range(r * 128, r * 128 + 128))
        rows += list(range(512 + r * 64, 512 + r * 64 + 64))
        rows += list(range(512 + (7 - r) * 64, 512 + (7 - r) * 64 + 64))
    return np.ascontiguousarray(w_out[rows][:, g * 256:(g + 1) * 256]).reshape(8, 128, 256)


def kernel(x, norm_w, w_in, b_igate, b_fgate, conv_w, conv_b, mlstm_norm_w, q_norm_w, k_norm_w, w_out, depth=None):
    x = np.asarray(x, np.float32)
    Bn, S, _ = x.shape
    depth = DEPTH if depth is None else depth
    P = dict(norm_w=norm_w, w_in=w_in, b_igate=b_igate, b_fgate=b_fgate, conv_w=conv_w, conv_b=conv_b,
             mlstm_norm_w=mlstm_norm_w, q_norm_w=q_norm_w, k_norm_w=k_norm_w, w_out=w_out)
    P = {k: np.asarray(v, np.float32) for k, v in P.items()}
    key = ('fused', S, depth)
    if key not in _CACHE:
        _CACHE[key] = build_fused(S, depth)
    nc = _CACHE[key]
    in_maps = []
    for c in range(8):
        b, g = c // 4, c % 4
        xT = np.ascontiguousarray(x[b].T)
        per = [_l1_inputs(S, g, xT, {k: v[l] for k, v in P.items()}) for l in range(depth)]
        in_maps.append({
            "xT": per[0]["xT"],
            "xo": np.ascontiguousarray(xT[g * 256:(g + 1) * 256].reshape(2, 128, S)),
            "w": np.stack([p["w"] for p in per], 0),
            "cst": np.stack([p["cst"] for p in per], 0),
            "wo": np.stack([_wo_perm(P['w_out'][l], g) for l in range(depth)], 0),
            "cstb": per[0]["cstb"], "oh": per[0]["oh"], "alr": per[0]["alr"]})
    res = run_bass_kernel_spmd(nc, in_maps, core_ids=list(range(8)))
    out = np.empty((Bn, S, D_MODEL), np.float32)
    for c in range(8):
        b, g = c // 4, c % 4
        out[b, :, g * 256:(g + 1) * 256] = np.asarray(res.results[c]["xn"]).reshape(256, S).T
    return out
```
